# Optimizing a Trainium2 kernel written in Bass

```python
import math
import jax, jax.numpy as jnp
from jax import lax
import numpy as np

D_MODEL = 1024
BATCH = 8
SEQ = 2048
DEPTH = 2

N_META = 16
EPS = 1e-6
ROPE_THETA = 10000.0
NEG_INF = -1e30
GLA_HEADS = 4
GLA_DK = 64
GLA_DV = 128
GLA_LOWRANK = 16
GLA_TAU = 16.0
GLA_CHUNK = 64
SWA_Q_HEADS = 8
SWA_KV_HEADS = 2
SWA_HEAD_DIM = 64
SWA_WINDOW = 128
SWA_BLOCK = 128
CONV_WIDTH = 31
CONV_CH = D_MODEL
D_FF = 3584
N_EXPERTS = 8
TOP_K = 2

GLA_QK = GLA_HEADS * GLA_DK
GLA_V = GLA_HEADS * GLA_DV
SWA_Q = SWA_Q_HEADS * SWA_HEAD_DIM
SWA_KV = SWA_KV_HEADS * SWA_HEAD_DIM
A_IN_WIDTHS = (GLA_QK, GLA_QK, GLA_V, GLA_V, GLA_LOWRANK, SWA_Q, SWA_KV, SWA_KV)
A_IN_COLS = sum(A_IN_WIDTHS)
A_SPLITS = tuple(int(s) for s in np.cumsum(A_IN_WIDTHS)[:-1])
MIX_OUT = GLA_V + SWA_Q
N_EVEN = (DEPTH + 1) // 2
N_ODD = DEPTH // 2

kernel_name = 'hybrid_gla_swa_conformer_moe'


def rms_norm(x, g):
    xf = x.astype(jnp.float32)
    y = xf * lax.rsqrt(jnp.mean(xf * xf, axis=-1, keepdims=True) + EPS)
    return (y * g.astype(jnp.float32)).astype(x.dtype)


def layer_norm(x, g, b):
    xf = x.astype(jnp.float32)
    mu = jnp.mean(xf, axis=-1, keepdims=True)
    var = jnp.mean(jnp.square(xf - mu), axis=-1, keepdims=True)
    y = (xf - mu) * lax.rsqrt(var + EPS)
    return (y * g.astype(jnp.float32) + b.astype(jnp.float32)).astype(x.dtype)


def rope_tables(length):
    inv_freq = 1.0 / (ROPE_THETA ** (jnp.arange(0, SWA_HEAD_DIM, 2, dtype=jnp.float32) / SWA_HEAD_DIM))
    ang = jnp.arange(length, dtype=jnp.float32)[:, None] * inv_freq[None, :]
    return jnp.cos(ang)[:, None, :], jnp.sin(ang)[:, None, :]


def apply_rope(x, cos, sin):
    half = x.shape[-1] // 2
    x1, x2 = x[..., :half], x[..., half:]
    cos = cos.astype(x.dtype)
    sin = sin.astype(x.dtype)
    return jnp.concatenate([x1 * cos - x2 * sin, x2 * cos + x1 * sin], axis=-1)


def gla_chunked(q, k, v, g):
    B, L, H, DK = q.shape
    DV = v.shape[-1]
    C = GLA_CHUNK
    pad = GLA_CHUNK - N_META
    n = (L + pad) // C

    def prep(t):
        t = jnp.pad(t.astype(jnp.float32), ((0, 0), (pad, 0), (0, 0), (0, 0)))
        return t.reshape(B, n, C, H, t.shape[-1]).transpose(0, 3, 1, 2, 4)

    q, k, v, g = prep(q) * (DK ** -0.5), prep(k), prep(v), prep(g)
    b = jnp.cumsum(g, axis=3)
    b_last = b[:, :, :, -1:, :]
    q_t = q * jnp.exp(b)
    k_t = k * jnp.exp(-b)
    causal = jnp.tril(jnp.ones((C, C), dtype=bool))
    att = jnp.where(causal, jnp.einsum('bhncd,bhnsd->bhncs', q_t, k_t), 0.0)
    o_intra = jnp.einsum('bhncs,bhnse->bhnce', att, v)
    kv = jnp.einsum('bhncd,bhnce->bhnde', k * jnp.exp(b_last - b), v)
    decay = jnp.exp(b_last[:, :, :, 0, :])

    def step(S, inp):
        dec_n, kv_n = inp
        return dec_n[..., None] * S + kv_n, S

    S0 = jnp.zeros((B, H, DK, DV), jnp.float32)
    _, S_in = lax.scan(step, S0, (jnp.moveaxis(decay, 2, 0), jnp.moveaxis(kv, 2, 0)))
    S_in = jnp.moveaxis(S_in, 0, 2)
    o = o_intra + jnp.einsum('bhncd,bhnde->bhnce', q_t, S_in)
    o = o.transpose(0, 2, 3, 1, 4).reshape(B, n * C, H, DV)
    return o[:, pad:]


def swa_sinks(q, k, v, sinks):
    B, L, HQ, D = q.shape
    HKV = k.shape[2]
    G = HQ // HKV
    W = SWA_BLOCK
    nb = -(-L // W)
    pad = nb * W - L

    def prep(t):
        return jnp.pad(t, ((0, 0), (pad, 0), (0, 0), (0, 0))).reshape(B, nb, W, t.shape[2], D)

    def with_prev(t):
        prev = jnp.pad(t, ((0, 0), (1, 0), (0, 0), (0, 0), (0, 0)))[:, :-1]
        return jnp.concatenate([prev, t], axis=2)

    qb = prep(q).reshape(B, nb, W, HKV, G, D)
    kk = with_prev(prep(k))
    vv = with_prev(prep(v))
    s = jnp.einsum('bnqhgd,bnkhd->bhgnqk', qb, kk).astype(jnp.float32) * (D ** -0.5)
    qi = jnp.arange(nb)[:, None] * W + jnp.arange(W)[None, :]
    ki = (jnp.arange(nb)[:, None] - 1) * W + jnp.arange(2 * W)[None, :]
    diff = qi[:, :, None] - ki[:, None, :]
    allowed = (diff >= 0) & (diff < SWA_WINDOW) & (ki[:, None, :] >= pad)
    s = jnp.where(allowed, s, NEG_INF)
    sink = jnp.broadcast_to(sinks.astype(jnp.float32).reshape(1, HKV, G, 1, 1, 1), s.shape[:-1] + (1,))
    p = jax.nn.softmax(jnp.concatenate([s, sink], axis=-1), axis=-1)[..., :-1]
    o = jnp.einsum('bhgnqk,bnkhd->bnqhgd', p.astype(v.dtype), vv)
    return o.reshape(B, nb * W, HQ, D)[:, pad:]


def mixer_gla_swa(h, w_in, w_gate2, b_gate, q_norm, k_norm, sinks, o_norm, w_out, cos, sin):
    B, L, _ = h.shape
    z = h @ w_in
    gq, gk, gv, gr, glr, sq, sk, sv = jnp.split(z, A_SPLITS, axis=-1)
    g = jax.nn.log_sigmoid((glr @ w_gate2 + b_gate).astype(jnp.float32)) / GLA_TAU
    o_a = gla_chunked(gq.reshape(B, L, GLA_HEADS, GLA_DK), gk.reshape(B, L, GLA_HEADS, GLA_DK),
                      gv.reshape(B, L, GLA_HEADS, GLA_DV), g.reshape(B, L, GLA_HEADS, GLA_DK))
    o_a = rms_norm(o_a.astype(h.dtype), o_norm).reshape(B, L, GLA_V) * jax.nn.silu(gr)
    q = apply_rope(rms_norm(sq.reshape(B, L, SWA_Q_HEADS, SWA_HEAD_DIM), q_norm), cos, sin)
    k = apply_rope(rms_norm(sk.reshape(B, L, SWA_KV_HEADS, SWA_HEAD_DIM), k_norm), cos, sin)
    o_b = swa_sinks(q, k, sv.reshape(B, L, SWA_KV_HEADS, SWA_HEAD_DIM), sinks).reshape(B, L, SWA_Q)
    return jnp.concatenate([o_a, o_b], axis=-1) @ w_out


def conformer_conv(h, w_pw1, w_dw, b_dw, ln_g, ln_b, w_pw2):
    a, gate = jnp.split(h @ w_pw1, 2, axis=-1)
    u = a * jax.nn.sigmoid(gate)
    u = lax.conv_general_dilated(u, w_dw[:, None, :].astype(u.dtype), window_strides=(1,),
                                 padding=[(CONV_WIDTH - 1, 0)],
                                 dimension_numbers=('NWC', 'WIO', 'NWC'),
                                 feature_group_count=CONV_CH) + b_dw
    u = jax.nn.silu(layer_norm(u, ln_g, ln_b))
    return u @ w_pw2


def swiglu(h, w1, w3, w2):
    return (jax.nn.silu(h @ w1) * (h @ w3)) @ w2


def moe_swiglu(h, w_router, w1, w3, w2):
    B, L, D = h.shape
    t = h.reshape(B * L, D)
    logits = (t @ w_router).astype(jnp.float32)
    top_v, top_i = lax.top_k(logits, TOP_K)
    gates = jax.nn.softmax(top_v, axis=-1)
    y = jnp.zeros_like(t)
    for e in range(N_EXPERTS):
        g_e = jnp.sum(jnp.where(top_i == e, gates, 0.0), axis=-1).astype(t.dtype)
        y = y + g_e[:, None] * swiglu(t, w1[e], w3[e], w2[e])
    return y.reshape(B, L, D)


def setup_inputs(seed: int = 0) -> dict:
    key = jax.random.key(seed)
    keys = list(jax.random.split(key, 32))

    def nrm(shape, scale):
        return jax.random.normal(keys.pop(), shape, jnp.float32) * scale

    def gain(shape):
        return 1.0 + 0.05 * jax.random.normal(keys.pop(), shape, jnp.float32)

    NE, NO, D, E = N_EVEN, N_ODD, D_MODEL, N_EXPERTS
    return {
        'x': nrm((BATCH, SEQ, D), 1.0),
        'meta': nrm((N_META, D), 1.0),
        'a_norm': gain((NE, D)),
        'a_w_in': nrm((NE, D, A_IN_COLS), D ** -0.5),
        'a_w_gate2': nrm((NE, GLA_LOWRANK, GLA_QK), GLA_LOWRANK ** -0.5),
        'a_b_gate': nrm((NE, GLA_QK), 0.1),
        'a_q_norm': gain((NE, SWA_HEAD_DIM)),
        'a_k_norm': gain((NE, SWA_HEAD_DIM)),
        'a_sinks': nrm((NE, SWA_Q_HEADS), 0.5),
        'a_o_norm': gain((NE, GLA_DV)),
        'a_w_out': nrm((NE, MIX_OUT, D), MIX_OUT ** -0.5),
        'f_norm': gain((NE, D)),
        'f_w1': nrm((NE, D, D_FF), D ** -0.5),
        'f_w3': nrm((NE, D, D_FF), D ** -0.5),
        'f_w2': nrm((NE, D_FF, D), D_FF ** -0.5),
        'c_norm': gain((NO, D)),
        'c_w_pw1': nrm((NO, D, 2 * CONV_CH), D ** -0.5),
        'c_w_dw': nrm((NO, CONV_WIDTH, CONV_CH), CONV_WIDTH ** -0.5),
        'c_b_dw': nrm((NO, CONV_CH), 0.02),
        'c_ln_g': gain((NO, CONV_CH)),
        'c_ln_b': nrm((NO, CONV_CH), 0.02),
        'c_w_pw2': nrm((NO, CONV_CH, D), CONV_CH ** -0.5),
        'm_norm': gain((NO, D)),
        'm_w_router': nrm((NO, D, E), D ** -0.5),
        'm_w1': nrm((NO, E, D, D_FF), D ** -0.5),
        'm_w3': nrm((NO, E, D, D_FF), D ** -0.5),
        'm_w2': nrm((NO, E, D_FF, D), D_FF ** -0.5),
    }


def reference(x, meta, a_norm, a_w_in, a_w_gate2, a_b_gate, a_q_norm, a_k_norm, a_sinks,
              a_o_norm, a_w_out, f_norm, f_w1, f_w3, f_w2, c_norm, c_w_pw1, c_w_dw, c_b_dw,
              c_ln_g, c_ln_b, c_w_pw2, m_norm, m_w_router, m_w1, m_w3, m_w2):
    B = x.shape[0]
    L = N_META + x.shape[1]
    h = jnp.concatenate([jnp.broadcast_to(meta[None].astype(x.dtype), (B, N_META, D_MODEL)), x], axis=1)
    cos, sin = rope_tables(L)
    for layer in range(DEPTH):
        j = layer // 2
        if layer % 2 == 0:
            h = h + mixer_gla_swa(rms_norm(h, a_norm[j]), a_w_in[j], a_w_gate2[j], a_b_gate[j],
                                  a_q_norm[j], a_k_norm[j], a_sinks[j], a_o_norm[j], a_w_out[j], cos, sin)
            h = h + swiglu(rms_norm(h, f_norm[j]), f_w1[j], f_w3[j], f_w2[j])
        else:
            h = h + conformer_conv(rms_norm(h, c_norm[j]), c_w_pw1[j], c_w_dw[j], c_b_dw[j],
                                   c_ln_g[j], c_ln_b[j], c_w_pw2[j])
            h = h + moe_swiglu(rms_norm(h, m_norm[j]), m_w_router[j], m_w1[j], m_w3[j], m_w2[j])
    return h[:, N_META:]
```

```python
import numpy as np
from contextlib import ExitStack
import concourse.bass as bass
import concourse.mybir as mybir
from concourse.bass_utils import run_bass_kernel_spmd

F32 = mybir.dt.float32
BF16 = mybir.dt.bfloat16
AF = mybir.ActivationFunctionType
ALU = mybir.AluOpType

D = 1024
NPOS = 2176
PAD = 112
NMETA = 16
SEQ = 2048
DFF = 3584
NE = 8
EPS = 1e-6
TILES_ALL = [(112, 16), (128, 512), (640, 512), (1152, 512), (1664, 512)]
TILES_REAL = TILES_ALL[1:]

V_ANORM, V_FNORM, V_CNORM, V_MNORM, V_CBDW, V_CLNG, V_CLNB = 0, 8, 16, 24, 32, 40, 48
V_WDW = 56
V_QN = V_WDW + 8 * 31
V_KN = V_QN + 1
V_ON = V_KN + 1
V_SINK = V_ON + 1
NV = V_SINK + 8
C_ROT, C_MC, C_GMASK, C_SMASK = 0, 128, 256, 384
C_COS = C_SMASK + 4 * 512
C_SIN = C_COS + NPOS
C_MC2 = C_SIN + NPOS
NC = C_MC2 + 128
JORD = [0, 2, 1, 3]
TILES_BLK = [(0, 128), (128, 512), (640, 512), (1152, 512), (1664, 512)]
O_GQ, O_GK, O_GV, O_GR, O_GLR, O_SQ, O_SK, O_SV = 0, 256, 512, 1024, 1536, 1552, 2064, 2192


class Ctx:
    ENG = ['pe', 'act', 'dve', 'pool', 'sp']

    def __init__(self, nc, es):
        self.nc = nc
        self.es = es
        self.eng = dict(pe=nc.tensor, act=nc.scalar, dve=nc.vector, pool=nc.gpsimd, sp=nc.sync)
        self.sem = {e: es.enter_context(nc.semaphore('c_' + e)) for e in self.ENG}
        self.cnt = {e: 0 for e in self.ENG}
        self.seen = {e: {} for e in self.ENG}
        self.lw = {}
        self.rd = {}
        self.dsem = {}
        self.dtot = {}
        self.nwait = 0
        self.ninst = 0

    def stream(self, name):
        if name not in self.dsem:
            self.dsem[name] = self.es.enter_context(self.nc.semaphore('d_' + name))
            self.dtot[name] = 0
        return name

    def _semval(self, tgt, val):
        if isinstance(tgt, tuple):
            return self.dsem[tgt[1]], max(val, self.dtot[tgt[1]])
        return self.sem[tgt], val

    def _waits(self, E, reads, writes):
        need = {}

        def add(tgt, val):
            if need.get(tgt, 0) < val:
                need[tgt] = val
        for k in reads:
            w = self.lw.get(k)
            if w is not None and not (w[0] == E and E == 'pe'):
                add(*w)
        for k in writes:
            w = self.lw.get(k)
            if w is not None and not (w[0] == E and E == 'pe'):
                add(*w)
            for t, v in self.rd.get(k, {}).items():
                if not (t == E and E == 'pe'):
                    add(t, v)
        for tgt, val in need.items():
            if tgt == E and val > self.cnt[E]:
                continue
            sem, val = self._semval(tgt, val)
            if self.seen[E].get(tgt, 0) >= val:
                continue
            self.eng[E].wait_ge(sem, val)
            self.seen[E][tgt] = val
            self.nwait += 1

    def op(self, E, fn, reads=(), writes=(), inc=True):
        self._waits(E, reads, writes)
        ins = fn(self.eng[E])
        idx = self.cnt[E] + 1
        if inc:
            ins.then_inc(self.sem[E], 1)
            self.cnt[E] = idx
        for k in writes:
            self.lw[k] = (E, idx)
            self.rd[k] = {}
        for k in reads:
            self.rd.setdefault(k, {})[E] = idx
        self.ninst += 1
        return ins

    def dma(self, Q, stream, out, in_, reads=(), writes=()):
        self.stream(stream)
        self._waits(Q, reads, writes)
        ins = self.eng[Q].dma_start(out=out, in_=in_)
        self.dtot[stream] += 16
        ins.then_inc(self.dsem[stream], 16)
        tgt = ('dma', stream)
        for k in writes:
            self.lw[k] = (tgt, self.dtot[stream])
            self.rd[k] = {}
        for k in reads:
            self.rd.setdefault(k, {})[tgt] = self.dtot[stream]
        return ins

    def barrier(self):
        for E in self.ENG:
            for F in self.ENG:
                if not (F == E and E in ('pe', 'sp')) and self.cnt[F] > self.seen[E].get(F, 0):
                    self.eng[E].wait_ge(self.sem[F], self.cnt[F])
                    self.seen[E][F] = self.cnt[F]
            for s in self.dsem:
                tgt = ('dma', s)
                if self.dtot[s] > self.seen[E].get(tgt, 0):
                    self.eng[E].wait_ge(self.dsem[s], self.dtot[s])
                    self.seen[E][tgt] = self.dtot[s]
        self.lw.clear()
        self.rd.clear()


def blk_keys(name, c, p0, n):
    return [(name, c, b) for b in range(p0 // 128, (p0 + n - 1) // 128 + 1)]


DBG = {}


class G:
    pass


def emit_rmsnorm(cx, g, vcol, tiles, xn, xn_name, sq, rstd_t, psn, xn32=None, nsq=8):
    nc = cx.nc
    for ti, (p0, n) in enumerate(tiles):
        pb = psn[ti % len(psn)]
        for c0 in range(0, 8, nsq):
            for c in range(c0, c0 + nsq):
                cx.op('act', lambda e: e.activation(out=sq[:, c % nsq, :n], in_=g.h[:, c, p0:p0 + n], func=AF.Square),
                      reads=blk_keys('h', c, p0, n), writes=[('sq', c % nsq)])
            for c in range(c0, c0 + nsq):
                cx.op('pe', lambda e: e.matmul(pb[1][:, :n], g.ones_bf[:], sq[:, c % nsq, :n], start=(c == 0), stop=(c == 7)),
                      reads=[('sq', c % nsq)], writes=[pb[0]], inc=True)
        rt = rstd_t[ti % 2]
        cx.op('act', lambda e: e.activation(out=rt[1][:, :n], in_=pb[1][:, :n], func=AF.Ln, bias=g.eps_ap, scale=1.0 / D),
              reads=[pb[0]], writes=[rt[0]])
        cx.op('act', lambda e: e.activation(out=rt[1][:, :n], in_=rt[1][:, :n], func=AF.Exp, scale=-0.5), reads=[rt[0]], writes=[rt[0]])
        for c in range(8):
            if xn32 is None:
                cx.op('dve', lambda e: e.scalar_tensor_tensor(out=xn[:, c, p0:p0 + n], in0=g.h[:, c, p0:p0 + n],
                                                               scalar=g.vecs[:, vcol + c:vcol + c + 1], in1=rt[1][:, :n],
                                                               op0=ALU.mult, op1=ALU.mult),
                      reads=blk_keys('h', c, p0, n) + [rt[0]], writes=blk_keys(xn_name, c, p0, n))
            else:
                x32 = xn32(ti)
                cx.op('dve', lambda e: e.scalar_tensor_tensor(out=x32[1][:, c, :n], in0=g.h[:, c, p0:p0 + n],
                                                               scalar=g.vecs[:, vcol + c:vcol + c + 1], in1=rt[1][:, :n],
                                                               op0=ALU.mult, op1=ALU.mult),
                      reads=blk_keys('h', c, p0, n) + [rt[0]], writes=[(x32[0], c)])
                cx.op('pool', lambda e: e.tensor_copy(out=xn[:, c, p0:p0 + n], in_=x32[1][:, c, :n]),
                      reads=[(x32[0], c)], writes=blk_keys(xn_name, c, p0, n))


def ffn_groups(w1, w3, w2, gate=None):
    w1v = w1.rearrange("(k p) f -> p k f", p=128)
    w3v = w3.rearrange("(k p) f -> p k f", p=128)
    return [dict(w1=w1v[:, :, j * 512:(j + 1) * 512], w3=w3v[:, :, j * 512:(j + 1) * 512],
                 w2=w2[j * 512:(j + 1) * 512, :].rearrange("(m p) d -> p m d", p=128), gate=gate)
            for j in range(DFF // 512)]


def ffn_load(cx, g, gr):
    s = g.wslot_n % 2
    g.wslot_n += 1
    cx.dma('pool', f'w{s}', g.w1g[s][:], gr['w1'], writes=[('w1g', s)])
    cx.dma('pool', f'w{s}', g.w3g[s][:], gr['w3'], writes=[('w3g', s)])
    cx.dma('pool', f'w{s}', g.w2g[s][:], gr['w2'], writes=[('w2g', s)])
    return s


def emit_ffn(cx, g, groups, tiles, xn, on_group=None, pre_step=None, first_slot=None):
    NG = len(groups)
    NT = len(tiles)

    def load(gi):
        return ffn_load(cx, g, groups[gi])

    slots = {}
    pending = None

    def down(gi, ti, hb):
        s = slots[gi]
        p0, n = tiles[ti]
        for d in range(8):
            pb = g.pso3[g.pso_n % 3]
            g.pso_n += 1
            for m in range(4):
                cx.op('pe', lambda e: e.matmul(pb[1][:, :n], g.w2g[s][:, m, d * 128:(d + 1) * 128], g.hh[hb][:, m, :n],
                                               start=(m == 0), stop=(m == 3)),
                      reads=[('w2g', s), ('hh', hb, m)], writes=[pb[0]], inc=(m == 3))
            cx.op('dve', lambda e: e.tensor_tensor(out=g.h[:, d, p0:p0 + n], in0=pb[1][:, :n], in1=g.h[:, d, p0:p0 + n], op=ALU.add),
                  reads=[pb[0]] + blk_keys('h', d, p0, n), writes=blk_keys('h', d, p0, n))

    slots[0] = first_slot if first_slot is not None else load(0)
    it = 0
    for gi in range(NG):
        gate = groups[gi]['gate']
        for ti in range(NT):
            s = slots[gi]
            p0, n = tiles[ti]
            hb = it % 2
            it += 1
            for m in range(4):
                p1 = g.ps1[g.ps1_n % 2]
                p3 = g.ps3[g.ps1_n % 2]
                g.ps1_n += 1
                for k in range(8):
                    cx.op('pe', lambda e: e.matmul(p1[1][:, :n], g.w1g[s][:, k, m * 128:(m + 1) * 128], xn[:, k, p0:p0 + n],
                                                   start=(k == 0), stop=(k == 7)),
                          reads=[('w1g', s)] + blk_keys('xn', k, p0, n), writes=[p1[0]], inc=(k == 7))
                for k in range(8):
                    cx.op('pe', lambda e: e.matmul(p3[1][:, :n], g.w3g[s][:, k, m * 128:(m + 1) * 128], xn[:, k, p0:p0 + n],
                                                   start=(k == 0), stop=(k == 7)),
                          reads=[('w3g', s)] + blk_keys('xn', k, p0, n), writes=[p3[0]], inc=(k == 7))
                sb = g.silu[g.silu_n % len(g.silu)]
                g.silu_n += 1
                cx.op('act', lambda e: e.activation(out=sb[1][:, :n], in_=p1[1][:, :n], func=AF.Silu),
                      reads=[p1[0]], writes=[sb[0]])
                if gate is not None:
                    cx.op('dve', lambda e: e.tensor_tensor(out=sb[1][:, :n], in0=p3[1][:, :n], in1=sb[1][:, :n], op=ALU.mult),
                          reads=[p3[0], sb[0]], writes=[sb[0]])
                    cx.op('pool', lambda e: e.tensor_tensor(out=g.hh[hb][:, m, :n], in0=sb[1][:, :n], in1=gate[1][:, p0 - 128:p0 - 128 + n], op=ALU.mult),
                          reads=[sb[0], gate[0] + (ti,)], writes=[('hh', hb, m)])
                else:
                    cx.op('dve', lambda e: e.tensor_tensor(out=g.hh[hb][:, m, :n], in0=p3[1][:, :n], in1=sb[1][:, :n], op=ALU.mult),
                          reads=[p3[0], sb[0]], writes=[('hh', hb, m)])
            if pending is not None:
                down(*pending)
            pending = (gi, ti, hb)
            if ti == 0 and gi + 1 < NG:
                slots[gi + 1] = load(gi + 1)
            if pre_step is not None:
                pre_step(gi, ti)
        if on_group is not None:
            on_group(gi)
    down(*pending)


def build_program(phases="ABCD", debug_out=False):
    nc = bass.Bass("TRN2", target_bir_lowering=False)
    dt_in = {}

    def din(name, shape):
        dt_in[name] = nc.dram_tensor(name, list(shape), F32, kind="ExternalInput").ap()
        return dt_in[name]

    xT = din("xT", [D, NPOS])
    vecs_d = din("vecs", [128, NV])
    ident_d = din("ident", [128, 128])
    if 'A' in phases:
        consts_d = din("consts", [128, NC])
        a_w_in = din("a_w_in", [D, 2320]); a_w_gate2 = din("a_w_gate2", [16, 256]); a_bgate = din("a_bgate", [1, 256])
        a_w_out = din("a_w_out", [D, D])
    if 'B' in phases:
        f_w1 = din("f_w1", [D, DFF]); f_w3 = din("f_w3", [D, DFF]); f_w2 = din("f_w2", [DFF, D])
    if 'C' in phases:
        c_w_pw1 = din("c_w_pw1", [D, 2 * D]); c_w_pw2 = din("c_w_pw2", [D, D])
    if 'D' in phases:
        m_w_router = din("m_w_router", [D, NE])
        m_w1 = din("m_w1", [NE, D, DFF]); m_w3 = din("m_w3", [NE, D, DFF]); m_w2 = din("m_w2", [NE, DFF, D])
    if debug_out:
        outT = nc.dram_tensor("outT", [D, NPOS], F32, kind="ExternalOutput").ap()
    else:
        outT = nc.dram_tensor("outT", [D, SEQ], F32, kind="ExternalOutput").ap()

    es = ExitStack()
    with es:
        cx = Ctx(nc, es)
        g = G()

        def sb(name, shape, dt, stack=es):
            return stack.enter_context(nc.sbuf_tensor(name, list(shape), dt))

        g.h = sb("h", [128, 8, NPOS], F32)
        g.vecs = sb("vecs_sb", [128, NV], F32)
        g.ident = sb("ident_sb", [128, 128], F32)
        g.ones_bf = sb("ones_bf", [128, 128], BF16)
        g.ones_f = sb("ones_f", [128, 128], F32)
        g.eps_t = sb("eps_t", [128, 1], F32)
        g.eps_ap = g.eps_t[:, 0:1]
        g.one_t = sb("one_t", [128, 1], F32)
        g.one_ap = g.one_t[:, 0:1]
        banks = [es.enter_context(nc.psum_tensor(f"psb{i}", [128, 512], F32)) for i in range(8)]
        g.ps1 = [(('ps', 0), banks[0]), (('ps', 1), banks[1])]
        g.ps3 = [(('ps', 2), banks[2]), (('ps', 3), banks[3])]
        g.pso = [(('ps', 4), banks[4]), (('ps', 5), banks[5])]
        g.psx = [(('ps', 6), banks[6]), (('ps', 7), banks[7])]
        g.pso3 = [g.pso[0], g.pso[1], g.psx[0]]
        g.ps1_n = g.pso_n = g.silu_n = g.wslot_n = 0

        hv = xT.rearrange("(c p) t -> p c t", p=128)
        cx.dma('sp', 'io', g.vecs[:], vecs_d, writes=['vecs'])
        cx.dma('sp', 'io', g.ident[:], ident_d, writes=['ident'])
        cx.op('dve', lambda e: e.memset(g.ones_bf[:], 1.0), writes=['ones_bf'])
        cx.op('dve', lambda e: e.memset(g.ones_f[:], 1.0), writes=['ones_f'])
        cx.op('dve', lambda e: e.memset(g.eps_t[:], EPS), writes=['eps'])
        cx.op('dve', lambda e: e.memset(g.one_t[:], 1.0), writes=['one'])
        cx.barrier()
        for ti, (p0, n) in enumerate(TILES_BLK):
            for c in range(8):
                cx.dma('sp', f'h{ti}', g.h[:, c, p0:p0 + n], hv[:, c, p0:p0 + n], writes=blk_keys('h', c, p0, n))

        if 'A' in phases:
            emit_phase_a(cx, g, sb, a_w_in, a_w_gate2, a_bgate, a_w_out, consts_d)

        if 'B' in phases:
            with ExitStack() as ps:
                xn = sb("xnB", [128, 8, NPOS], BF16, ps)
                sq = sb("sqB", [128, 8, 512], BF16, ps)
                rstd = [(('rstd', i), sb(f"rstdB{i}", [128, 512], F32, ps)) for i in range(2)]
                g.w1g = [sb(f"w1g{i}", [128, 8, 512], BF16, ps) for i in range(2)]
                g.w3g = [sb(f"w3g{i}", [128, 8, 512], BF16, ps) for i in range(2)]
                g.w2g = [sb(f"w2g{i}", [128, 4, 1024], BF16, ps) for i in range(2)]
                g.hh = [sb(f"hh{i}", [128, 4, 512], BF16, ps) for i in range(2)]
                g.silu = [(('silu', i), sb(f"silu{i}", [128, 512], F32, ps)) for i in range(2)]
                groups_b = ffn_groups(f_w1, f_w3, f_w2)
                slot0 = ffn_load(cx, g, groups_b[0])

                def norm_b(ti):
                    emit_rmsnorm(cx, g, V_FNORM, [TILES_ALL[ti]], xn, 'xn', sq, [rstd[ti % 2]], [g.psx[1]])

                def pre_b(gi, ti):
                    if gi == 0 and ti + 1 < len(TILES_ALL):
                        norm_b(ti + 1)
                norm_b(0)
                emit_ffn(cx, g, groups_b, TILES_ALL, xn, pre_step=pre_b, first_slot=slot0)
                cx.barrier()

        if 'C' in phases:
            emit_phase_c(cx, g, sb, c_w_pw1, c_w_pw2)

        if 'D' in phases:
            emit_phase_d(cx, g, sb, m_w_router, m_w1, m_w3, m_w2)

        ov = outT.rearrange("(c p) t -> p c t", p=128)
        for c in range(8):
            if debug_out:
                cx.dma('sp', 'io', ov[:, c, :], g.h[:, c, :], reads=[('h', c, b) for b in range(17)])
            else:
                cx.dma('sp', 'io', ov[:, c, :], g.h[:, c, 128:NPOS], reads=[('h', c, b) for b in range(17)])
        cx.barrier()
        g.stats = (cx.ninst, cx.nwait)
        g.in_names = list(dt_in.keys())
    return nc, g


def emit_phase_a(cx, g, sb, w_in, w_gate2, bgate, w_out, consts):
    nc = cx.nc
    wv = w_in.rearrange("(k p) f -> p k f", p=128)
    B = [g.ps1[0], g.ps1[1], g.ps3[0], g.ps3[1], g.pso[0], g.pso[1], g.psx[0], g.psx[1]]
    with ExitStack() as pa:
        xn = sb("xnA", [128, 8, NPOS], BF16, pa)
        mix = sb("mixA", [128, 8, NPOS], BF16, pa)
        with ExitStack() as p01:
            wglr = sb("wglr", [128, 8, 16], BF16, p01)
            wg2 = sb("wg2", [16, 256], F32, p01)
            bg = sb("bgA", [1, 256], F32, p01)
            mc = sb("mcA", [128, 128], F32, p01)
            gmask = sb("gmaskA", [128, 128], F32, p01)
            mc2 = sb("mc2A", [128, 128], F32, p01)
            wq = sb("wqA", [128, 8, 128], BF16, p01)
            wk = sb("wkA", [128, 8, 128], BF16, p01)
            wvv = sb("wvA", [128, 8, 256], BF16, p01)
            cx.dma('pool', 'w0', wglr[:], wv[:, :, O_GLR:O_GLR + 16], writes=['wglr'])
            cx.dma('sp', 'io', wg2[:], w_gate2, writes=['wg2'])
            cx.dma('sp', 'io', bg[:], bgate, writes=['bg'])
            cx.dma('sp', 'io', mc[:], consts[:, C_MC:C_MC + 128], writes=['mc'])
            cx.dma('sp', 'io', gmask[:], consts[:, C_GMASK:C_GMASK + 128], writes=['gmask'])
            cx.dma('sp', 'io', mc2[:], consts[:, C_MC2:C_MC2 + 128], writes=['mc2'])
            cx.dma('pool', 'w1', wq[:], wv[:, :, O_GQ:O_GQ + 128], writes=['wq'])
            cx.dma('pool', 'w1', wk[:], wv[:, :, O_GK:O_GK + 128], writes=['wk'])
            cx.dma('pool', 'w1', wvv[:], wv[:, :, O_GV:O_GV + 256], writes=['wvv'])
            with ExitStack() as ps:
                sq = sb("sqA", [128, 8, 512], BF16, ps)
                rstd = [(('rstd', i), sb(f"rstdA{i}", [128, 512], F32, ps)) for i in range(2)]
                cx.op('pool', lambda e: e.memset(xn[:, :, 0:PAD], 0.0), writes=[('xn', c, 0) for c in range(8)])
                emit_rmsnorm(cx, g, V_ANORM, TILES_ALL, xn, 'xn', sq, rstd, g.psx)
                cx.barrier()
            if DBG.get('a_cut', 99) <= 0:
                return
            with ExitStack() as ps:
                glrT = sb("glrT", [16, NPOS], F32, ps)
                ebT = sb("ebT", [128, NPOS], F32, ps)
                qtT = sb("qtT", [128, NPOS], BF16, ps)
                ktT = sb("ktT", [128, NPOS], BF16, ps)
                kttok = sb("kttok", [128, 17, 128], BF16, ps)
                vtok = sb("vtok", [128, 17, 256], BF16, ps)
                srT = sb("srT", [128, 2, NPOS], BF16, ps)
                wr = wvv
                sp = [(('sp', i), sb(f"spA{i}", [128, 128], F32, ps)) for i in range(2)]
                e1 = [(('e1', i), sb(f"e1A{i}", [128, 128], F32, ps)) for i in range(2)]
                emb = [(('emb', i), sb(f"embA{i}", [128, 128], F32, ps)) for i in range(2)]
                rec = [(('rec', i), sb(f"recA{i}", [128, 512], F32, ps)) for i in range(2)]
                decT = sb("decT", [128, 34], F32, ps)
                attm = [(('attm', i), sb(f"attmA{i}", [128, 128], BF16, ps)) for i in range(2)]
                S32 = [sb(f"S32_{i}", [128, 128], F32, ps) for i in range(2)]
                Sbf = [[sb(f"Sbf{i}_{k}", [128, 128], BF16, ps) for k in range(2)] for i in range(2)]
                osq = [(('osq', i), sb(f"osqA{i}", [128, 256], BF16, ps)) for i in range(2)]
                ort = ([('ebT', 0, 0), ('ebT', 0, 1)], ebT[:, 0:256])
                ot = ([('ebT', 0, 2), ('ebT', 0, 3)], ebT[:, 256:512])
                for ti, (p0, n) in enumerate(TILES_BLK):
                    pb = B[ti % 2]
                    for k in range(8):
                        cx.op('pe', lambda e: e.matmul(pb[1][0:16, :n], wglr[:, k, :], xn[:, k, p0:p0 + n], start=(k == 0), stop=(k == 7)),
                              reads=['wglr'] + blk_keys('xn', k, p0, n), writes=[pb[0]], inc=(k == 7))
                    cx.op('act', lambda e: e.activation(out=glrT[:, p0:p0 + n], in_=pb[1][0:16, :n], func=AF.Copy),
                          reads=[pb[0]], writes=blk_keys('glrT', 0, p0, n))
                if DBG.get('a_cut', 99) <= 1:
                    return
                for p in range(2):
                    if p > 0:
                        cx.dma('pool', 'w1', wq[:], wv[:, :, O_GQ + 128 * p:O_GQ + 128 * p + 128], writes=['wq'])
                        cx.dma('pool', 'w1', wk[:], wv[:, :, O_GK + 128 * p:O_GK + 128 * p + 128], writes=['wk'])
                        cx.dma('pool', 'w1', wvv[:], wv[:, :, O_GV + 256 * p:O_GV + 256 * p + 256], writes=['wvv'])
                    for b in range(17):
                        q0 = 128 * b
                        pg, pbt, pbT, pk, pvv = B[0], B[1], B[2], B[3], B[4 + b % 2]
                        spb, e1b, embb = sp[b % 2], e1[b % 2], emb[b % 2]
                        cx.op('pe', lambda e: e.matmul(pg[1][:, 0:128], glrT[:, q0:q0 + 128], wg2[:, 128 * p:128 * p + 128], start=True, stop=False),
                              reads=[('glrT', 0, b), 'wg2'], writes=[pg[0]], inc=False)
                        cx.op('pe', lambda e: e.matmul(pg[1][:, 0:128], g.ones_f[0:1, :], bg[0:1, 128 * p:128 * p + 128], start=False, stop=True),
                              reads=['bg'], writes=[pg[0]])
                        cx.op('act', lambda e: e.activation(out=e1b[1][:], in_=pg[1][:, 0:128], func=AF.Exp, scale=-1.0),
                              reads=[pg[0]], writes=[e1b[0]])
                        cx.op('act', lambda e: e.activation(out=spb[1][:], in_=e1b[1][:], func=AF.Ln, bias=g.one_ap, scale=1.0),
                              reads=[e1b[0]], writes=[spb[0]])
                        if b == 0:
                            cx.op('pool', lambda e: e.memset(spb[1][0:64, :], 0.0), reads=[spb[0]], writes=[spb[0]])
                            cx.op('pool', lambda e: e.memset(spb[1][64:PAD, :], 0.0), reads=[spb[0]], writes=[spb[0]])
                        cx.op('pe', lambda e: e.matmul(pbt[1][:, 0:128], mc2[:], spb[1][:], start=True, stop=True),
                              reads=['mc2', spb[0]], writes=[pbt[0]])
                        cx.op('act', lambda e: e.activation(out=embb[1][:], in_=pbt[1][:, 0:128], func=AF.Exp, scale=1.0),
                              reads=[pbt[0]], writes=[embb[0]])
                        cx.op('pe', lambda e: e.matmul(pbT[1][:, 0:128], spb[1][:], mc[:], start=True, stop=True),
                              reads=['mc', spb[0]], writes=[pbT[0]])
                        cx.op('act', lambda e: e.activation(out=ebT[:, q0:q0 + 128], in_=pbT[1][:, 0:128], func=AF.Copy),
                              reads=[pbT[0]], writes=[('ebT', 0, b)])
                        for hf in range(2):
                            cx.op('act', lambda e: e.activation(out=decT[:, 2 * b + hf:2 * b + hf + 1], in_=pbT[1][:, 64 * hf + 63:64 * hf + 64], func=AF.Exp),
                                  reads=[pbT[0]], writes=['decT'], inc=(hf == 1))
                        for k in range(8):
                            cx.op('pe', lambda e: e.matmul(pk[1][:, 0:128], xn[:, k, q0:q0 + 128], wk[:, k, :], start=(k == 0), stop=(k == 7)),
                                  reads=['wk', ('xn', k, b)], writes=[pk[0]], inc=(k == 7))
                        cx.op('dve', lambda e: e.tensor_tensor(out=kttok[:, b, :], in0=pk[1][:, 0:128], in1=embb[1][:], op=ALU.mult),
                              reads=[pk[0], embb[0]], writes=[('kttok', b)])
                        for k in range(8):
                            cx.op('pe', lambda e: e.matmul(pvv[1][:, 0:256], xn[:, k, q0:q0 + 128], wvv[:, k, :], start=(k == 0), stop=(k == 7)),
                                  reads=['wvv', ('xn', k, b)], writes=[pvv[0]], inc=(k == 7))
                        cx.op('act', lambda e: e.activation(out=vtok[:, b, :], in_=pvv[1][:, 0:256], func=AF.Copy),
                              reads=[pvv[0]], writes=[('vtok', b)])
                    if DBG.get('a_cut', 99) <= 2:
                        continue
                    cx.dma('pool', 'w1', wr[:], wv[:, :, O_GR + 256 * p:O_GR + 256 * p + 256], writes=['wvv'])
                    for ti, (p0, n) in enumerate(TILES_BLK):
                        pq, pkk = B[6], B[7]
                        rc, rc2 = rec[0], rec[1]
                        for k in range(8):
                            cx.op('pe', lambda e: e.matmul(pq[1][:, :n], wq[:, k, :], xn[:, k, p0:p0 + n], start=(k == 0), stop=(k == 7)),
                                  reads=['wq'] + blk_keys('xn', k, p0, n), writes=[pq[0]], inc=(k == 7))
                        cx.op('act', lambda e: e.activation(out=rc[1][:, :n], in_=ebT[:, p0:p0 + n], func=AF.Exp),
                              reads=blk_keys('ebT', 0, p0, n), writes=[rc[0]])
                        cx.op('dve', lambda e: e.scalar_tensor_tensor(out=qtT[:, p0:p0 + n], in0=pq[1][:, :n], scalar=0.125, in1=rc[1][:, :n],
                                                                       op0=ALU.mult, op1=ALU.mult),
                              reads=[pq[0], rc[0]], writes=blk_keys('qtT', 0, p0, n))
                        for k in range(8):
                            cx.op('pe', lambda e: e.matmul(pkk[1][:, :n], wk[:, k, :], xn[:, k, p0:p0 + n], start=(k == 0), stop=(k == 7)),
                                  reads=['wk'] + blk_keys('xn', k, p0, n), writes=[pkk[0]], inc=(k == 7))
                        cx.op('act', lambda e: e.activation(out=rc2[1][:, :n], in_=ebT[:, p0:p0 + n], func=AF.Exp, scale=-1.0),
                              reads=blk_keys('ebT', 0, p0, n), writes=[rc2[0]])
                        cx.op('dve', lambda e: e.tensor_tensor(out=ktT[:, p0:p0 + n], in0=pkk[1][:, :n], in1=rc2[1][:, :n], op=ALU.mult),
                              reads=[pkk[0], rc2[0]], writes=blk_keys('ktT', 0, p0, n))
                        for j in range(2):
                            pr = B[4 + j]
                            for k in range(8):
                                cx.op('pe', lambda e: e.matmul(pr[1][:, :n], wr[:, k, 128 * j:128 * j + 128], xn[:, k, p0:p0 + n], start=(k == 0), stop=(k == 7)),
                                      reads=['wvv'] + blk_keys('xn', k, p0, n), writes=[pr[0]], inc=(k == 7))
                            cx.op('act', lambda e: e.activation(out=srT[:, j, p0:p0 + n], in_=pr[1][:, :n], func=AF.Silu),
                                  reads=[pr[0]], writes=blk_keys('srT', j, p0, n))
                    if DBG.get('a_cut', 99) <= 3:
                        continue
                    cx.op('dve', lambda e: e.memset(S32[0][:], 0.0), writes=[('S32', 0)])
                    cx.op('pool', lambda e: e.memset(Sbf[0][0][:], 0.0), writes=[('Sbf', 0, 0)])

                    def Oap(b):
                        sl = b % 4
                        return B[sl % 2][0], B[sl % 2][1][:, 256 * (sl // 2):256 * (sl // 2) + 256]

                    def g_att(b):
                        q0 = 128 * b
                        for j in range(2):
                            pat = B[2 + j]
                            am = attm[j]
                            cx.op('pe', lambda e: e.matmul(pat[1][:, 0:128], ktT[64 * j:64 * j + 64, q0:q0 + 128], qtT[64 * j:64 * j + 64, q0:q0 + 128], start=True, stop=True),
                                  reads=[('ktT', 0, b), ('qtT', 0, b)], writes=[pat[0]])
                            cx.op('dve', lambda e: e.tensor_tensor(out=am[1][:], in0=pat[1][:, 0:128], in1=gmask[:], op=ALU.mult),
                                  reads=[pat[0], 'gmask'], writes=[am[0]])

                    def g_kv(b, hf):
                        pkv = B[4 + hf]
                        cx.op('pe', lambda e: e.matmul(pkv[1][:, 0:256], kttok[64 * hf:64 * hf + 64, b, :], vtok[64 * hf:64 * hf + 64, b, :], start=True, stop=True),
                              reads=[('kttok', b), ('vtok', b)], writes=[pkv[0]])

                    def g_upd(b, hf):
                        pkv = B[4 + hf]
                        ci = 2 * b + hf
                        dec = decT[:, ci:ci + 1]
                        src, dst = S32[ci % 2], S32[(ci + 1) % 2]
                        sk, dk = ('S32', ci % 2), ('S32', (ci + 1) % 2)
                        for j in range(2):
                            cx.op('dve', lambda e: e.scalar_tensor_tensor(out=dst[64 * j:64 * j + 64, :], in0=src[64 * j:64 * j + 64, :],
                                                                           scalar=decT[64 * j:64 * j + 64, ci:ci + 1], in1=pkv[1][64 * j:64 * j + 64, 128 * j:128 * j + 128],
                                                                           op0=ALU.mult, op1=ALU.add),
                                  reads=[pkv[0], sk, 'decT'], writes=[dk], inc=(j == 1))
                        sbk = (1, b % 2) if hf == 0 else (0, (b + 1) % 2)
                        cx.op('act', lambda e: e.activation(out=Sbf[sbk[0]][sbk[1]][:], in_=dst[:], func=AF.Copy), reads=[dk], writes=[('Sbf',) + sbk])

                    def g_omm(b):
                        q0 = 128 * b
                        ok, oap = Oap(b)
                        for j in range(2):
                            am = attm[j]
                            cx.op('pe', lambda e: e.matmul(oap[:, 128 * j:128 * j + 128], vtok[:, b, 128 * j:128 * j + 128], am[1][:], start=True, stop=False),
                                  reads=[('vtok', b), am[0]], writes=[ok], inc=False)
                            for hf in range(2):
                                c0 = q0 + 64 * hf
                                cx.op('pe', lambda e: e.matmul(oap[:, 128 * j + 64 * hf:128 * j + 64 * hf + 64], Sbf[hf][b % 2][64 * j:64 * j + 64, :], qtT[64 * j:64 * j + 64, c0:c0 + 64],
                                                               start=False, stop=(hf == 1)),
                                      reads=[('Sbf', hf, b % 2), ('qtT', 0, b)], writes=[ok], inc=(hf == 1))

                    def g_sq(b):
                        ok, oap = Oap(b)
                        os_ = osq[b % 2]
                        cx.op('act', lambda e: e.activation(out=os_[1][:], in_=oap, func=AF.Square), reads=[ok], writes=[os_[0]])

                    def g_ones(b):
                        pss = B[6 + b % 2]
                        os_ = osq[b % 2]
                        cx.op('pe', lambda e: e.matmul(pss[1][:, 0:256], g.ones_bf[:], os_[1][:], start=True, stop=True), reads=[os_[0]], writes=[pss[0]])

                    def g_fin(b):
                        q0 = 128 * b
                        ok, oap = Oap(b)
                        pss = B[6 + b % 2]
                        cx.op('act', lambda e: e.activation(out=ort[1], in_=pss[1][:, 0:256], func=AF.Ln, bias=g.eps_ap, scale=1.0 / 128),
                              reads=[pss[0]], writes=ort[0])
                        cx.op('act', lambda e: e.activation(out=ort[1], in_=ort[1], func=AF.Exp, scale=-0.5), reads=ort[0], writes=ort[0])
                        cx.op('dve', lambda e: e.scalar_tensor_tensor(out=ot[1], in0=oap, scalar=g.vecs[:, V_ON:V_ON + 1], in1=ort[1],
                                                                       op0=ALU.mult, op1=ALU.mult),
                              reads=[ok] + ort[0], writes=ot[0])
                        cx.op('pool', lambda e: e.tensor_tensor(out=mix[:, 2 * p:2 * p + 2, q0:q0 + 128], in0=ot[1].rearrange("p (j c) -> p j c", j=2),
                                                                in1=srT[:, :, q0:q0 + 128], op=ALU.mult),
                              reads=ot[0] + [('srT', 0, b), ('srT', 1, b)], writes=[('mix', 2 * p, b), ('mix', 2 * p + 1, b)])

                    g_att(0)
                    g_kv(0, 0)
                    g_kv(0, 1)
                    for it in range(17 + 3):
                        b = it
                        if b < 17:
                            g_upd(b, 0)
                            if b + 1 < 17:
                                g_kv(b + 1, 0)
                            g_omm(b)
                            g_upd(b, 1)
                            if b + 1 < 17:
                                g_kv(b + 1, 1)
                                g_att(b + 1)
                        if 0 <= it - 1 < 17:
                            g_sq(it - 1)
                        if 0 <= it - 2 < 17:
                            g_ones(it - 2)
                        if 0 <= it - 3 < 17:
                            g_fin(it - 3)
                cx.barrier()
            if DBG.get('a_cut', 99) <= 4:
                return
        with ExitStack() as ps:
            rot = sb("rotA", [128, 128], F32, ps)
            bd = sb("bdA", [128, 128], BF16, ps)
            smask = sb("smaskA", [128, 4, 128], F32, ps)
            cosT = sb("cosA", [128, NPOS], F32, ps)
            sinT = sb("sinA", [128, NPOS], F32, ps)
            sinkexp = sb("sinkexpA", [128, 8], F32, ps)
            qr = sb("qrA", [128, 2, NPOS], BF16, ps)
            kr = sb("krA", [128, NPOS], BF16, ps)
            svt = sb("svtA", [128, 17, 128], BF16, ps)
            wsq = sb("wsqA", [128, 8, 256], BF16, ps)
            wsk = sb("wskA", [128, 8, 128], BF16, ps)
            wsv = sb("wsvA", [128, 8, 128], BF16, ps)
            zsq = [(('zsq', i), sb(f"zsqA{i}", [128, 512], BF16, ps)) for i in range(2)]
            zrt = [(('zrt', i), sb(f"zrtA{i}", [128, 512], F32, ps)) for i in range(2)]
            qn = [(('qn', i), sb(f"qnA{i}", [128, 512], F32, ps)) for i in range(2)]
            t1 = [(('t1', i), sb(f"t1A{i}", [128, 512], F32, ps)) for i in range(1)]
            t2 = [(('t2', i), sb(f"t2A{i}", [128, 512], F32, ps)) for i in range(1)]
            identbf = sb("identbfA", [128, 128], BF16, ps)
            mb = sb("mbA", [128, 4, 256], BF16, ps)
            em = [(('em', i), sb(f"emA{i}", [128, 512], BF16, ps)) for i in range(2)]
            dns = [(('dns', i), sb(f"dnsA{i}", [128, 512], F32, ps)) for i in range(2)]
            cx.dma('sp', 'io', rot[:], consts[:, C_ROT:C_ROT + 128], writes=['rot'])
            cx.dma('sp', 'io', smask[:], consts[:, C_SMASK:C_SMASK + 2048].rearrange("p (t c) -> p t c", t=4)[:, :, 0:128], writes=['smask'])
            cx.dma('sp', 'io', cosT[:], consts[:, C_COS:C_COS + NPOS], writes=['cos'])
            cx.dma('sp', 'io', sinT[:], consts[:, C_SIN:C_SIN + NPOS], writes=['sin'])
            cx.op('pool', lambda e: e.memset(bd[:], 0.0), writes=['bd'])
            cx.op('pool', lambda e: e.memset(bd[0:64, 0:64], 1.0), reads=['bd'], writes=['bd'])
            cx.op('pool', lambda e: e.memset(bd[64:128, 64:128], 1.0), reads=['bd'], writes=['bd'])
            cx.op('act', lambda e: e.activation(out=sinkexp[:], in_=g.vecs[:, V_SINK:V_SINK + 8], func=AF.Exp), writes=['sinkexp'])
            cx.op('dve', lambda e: e.tensor_copy(out=identbf[:], in_=g.ident[:]), writes=['identbf'])
            for r in range(2):
                cx.op('dve', lambda e: e.tensor_scalar(out=mb[:, :, 128 * r:128 * r + 128], in0=smask[:], scalar1=-1.0, scalar2=30000.0, op0=ALU.add, op1=ALU.mult),
                      reads=['smask'], writes=['mb'])
            for hk in range(2):
                cx.dma('pool', 'w0', wsq[:], wv[:, :, O_SQ + 256 * hk:O_SQ + 256 * hk + 256], writes=['wsq'])
                for r in range(2):
                    cx.dma('pool', 'w0', wsk[:, :, 64 * r:64 * r + 64], wv[:, :, O_SK + 64 * hk:O_SK + 64 * hk + 64], writes=['wsk'])
                    cx.dma('pool', 'w0', wsv[:, :, 64 * r:64 * r + 64], wv[:, :, O_SV + 64 * hk:O_SV + 64 * hk + 64], writes=['wsv'])
                its = [(ci, p0, n) for ci in range(3) for (p0, n) in TILES_BLK]
                PZ, PSS, PROT = [B[0], B[1], B[2]], [B[3], B[4]], B[5]

                def q_z(i):
                    ci, p0, n = its[i]
                    pz = PZ[i % 3]
                    for k in range(8):
                        lhs = wsq[:, k, 128 * ci:128 * ci + 128] if ci < 2 else wsk[:, k, :]
                        cx.op('pe', lambda e: e.matmul(pz[1][:, :n], lhs, xn[:, k, p0:p0 + n], start=(k == 0), stop=(k == 7)),
                              reads=['wsq', 'wsk'] + blk_keys('xn', k, p0, n), writes=[pz[0]], inc=(k == 7))

                def q_mid(i):
                    ci, p0, n = its[i]
                    pz, pss = PZ[i % 3], PSS[i % 2]
                    zs, zr, qq = zsq[i % 2], zrt[i % 2], qn[i % 2]
                    cx.op('act', lambda e: e.activation(out=zs[1][:, :n], in_=pz[1][:, :n], func=AF.Square), reads=[pz[0]], writes=[zs[0]])
                    cx.op('pe', lambda e: e.matmul(pss[1][:, :n], bd[:], zs[1][:, :n], start=True, stop=True), reads=['bd', zs[0]], writes=[pss[0]])
                    cx.op('act', lambda e: e.activation(out=zr[1][:, :n], in_=pss[1][:, :n], func=AF.Ln, bias=g.eps_ap, scale=1.0 / 64),
                          reads=[pss[0]], writes=[zr[0]])
                    cx.op('act', lambda e: e.activation(out=zr[1][:, :n], in_=zr[1][:, :n], func=AF.Exp, scale=-0.5), reads=[zr[0]], writes=[zr[0]])
                    gcol = V_QN if ci < 2 else V_KN
                    cx.op('dve', lambda e: e.scalar_tensor_tensor(out=qq[1][:, :n], in0=pz[1][:, :n], scalar=g.vecs[:, gcol:gcol + 1], in1=zr[1][:, :n],
                                                                   op0=ALU.mult, op1=ALU.mult),
                          reads=[pz[0], zr[0]], writes=[qq[0]])

                def q_tail(i):
                    ci, p0, n = its[i]
                    qq, a1, a2 = qn[i % 2], t1[0], t2[0]
                    cx.op('pe', lambda e: e.matmul(PROT[1][:, :n], rot[:], qq[1][:, :n], start=True, stop=True), reads=['rot', qq[0]], writes=[PROT[0]])
                    cx.op('dve', lambda e: e.tensor_tensor(out=a1[1][:, :n], in0=qq[1][:, :n], in1=cosT[:, p0:p0 + n], op=ALU.mult),
                          reads=[qq[0], 'cos'], writes=[a1[0]])
                    cx.op('dve', lambda e: e.tensor_tensor(out=a2[1][:, :n], in0=PROT[1][:, :n], in1=sinT[:, p0:p0 + n], op=ALU.mult),
                          reads=[PROT[0], 'sin'], writes=[a2[0]])
                    dst = qr[:, ci, p0:p0 + n] if ci < 2 else kr[:, p0:p0 + n]
                    dkeys = blk_keys('qr', ci, p0, n) if ci < 2 else blk_keys('kr', 0, p0, n)
                    cx.op('pool', lambda e: e.tensor_tensor(out=dst, in0=a1[1][:, :n], in1=a2[1][:, :n], op=ALU.add),
                          reads=[a1[0], a2[0]], writes=dkeys)

                def q_v(b):
                    pv = B[6 + b % 2]
                    for k in range(8):
                        cx.op('pe', lambda e: e.matmul(pv[1][:, 0:128], xn[:, k, 128 * b:128 * b + 128], wsv[:, k, :], start=(k == 0), stop=(k == 7)),
                              reads=['wsv', ('xn', k, b)], writes=[pv[0]], inc=(k == 7))
                    cx.op('act', lambda e: e.activation(out=svt[:, b, :], in_=pv[1][:, 0:128], func=AF.Copy), reads=[pv[0]], writes=[('svt', b)])

                q_z(0)
                for i in range(len(its)):
                    if i + 1 < len(its):
                        q_z(i + 1)
                    q_mid(i)
                    if i >= 1:
                        q_tail(i - 1)
                    q_v(i)
                q_tail(len(its) - 1)
                q_v(15)
                q_v(16)
                if DBG.get('a_cut', 99) <= 6:
                    continue
                units = []
                for qb in range(17):
                    kbs = [qb - 1, qb] if qb >= 1 else [0]
                    for idx, kb in enumerate(kbs):
                        units.append((qb, kb, idx, len(kbs)))

                def s_scores(n):
                    qb, kb, idx, nk = units[n]
                    q0, k0 = 128 * qb, 128 * kb
                    scA, scB = B[(2 * n) % 4], B[(2 * n + 1) % 4]
                    mt = (0 if kb == qb else 1) + (2 if kb == 0 else 0)
                    for c, j in enumerate(JORD):
                        hp = 64 * (j % 2)
                        sc = scA if j % 2 == 0 else scB
                        cc = c % 2
                        cx.op('pe', lambda e: e.matmul(sc[1][:, 128 * cc:128 * cc + 128], kr[hp:hp + 64, k0:k0 + 128], qr[hp:hp + 64, j // 2, q0:q0 + 128],
                                                       start=(cc == 0), stop=False),
                              reads=[('kr', 0, kb), ('qr', j // 2, qb)], writes=[sc[0]], inc=False)
                    for sc in (scA, scB):
                        cx.op('pe', lambda e: e.matmul(sc[1][:, 0:256], identbf[:], mb[:, mt, :], start=False, stop=True),
                              reads=['identbf', 'mb'], writes=[sc[0]])

                def s_pv(n):
                    qb, kb, idx, nk = units[n]
                    scA, scB = B[(2 * n) % 4], B[(2 * n + 1) % 4]
                    Oo, Dn = B[4 + qb % 2], B[6 + qb % 2]
                    emm = em[n % 2]
                    cx.op('act', lambda e: e.activation(out=emm[1][:, 0:256], in_=scA[1][:, 0:256], func=AF.Exp, scale=0.125), reads=[scA[0]], writes=[emm[0]])
                    cx.op('act', lambda e: e.activation(out=emm[1][:, 256:512], in_=scB[1][:, 0:256], func=AF.Exp, scale=0.125), reads=[scB[0], emm[0]], writes=[emm[0]])
                    cx.op('pe', lambda e: e.matmul(Oo[1][:], svt[:, kb, :], emm[1][:], start=(idx == 0), stop=(idx == nk - 1)),
                          reads=[('svt', kb), emm[0]], writes=[Oo[0]])
                    cx.op('pe', lambda e: e.matmul(Dn[1][:], g.ones_bf[:], emm[1][:], start=(idx == 0), stop=(idx == nk - 1)),
                          reads=[emm[0]], writes=[Dn[0]])

                def s_fin1(qb):
                    Dn = B[6 + qb % 2]
                    dn = dns[qb % 2]
                    for c, j in enumerate(JORD):
                        cx.op('dve', lambda e: e.tensor_scalar(out=dn[1][:, 128 * c:128 * c + 128], in0=Dn[1][:, 128 * c:128 * c + 128],
                                                                scalar1=sinkexp[:, 4 * hk + j:4 * hk + j + 1], scalar2=None, op0=ALU.add),
                              reads=[Dn[0], 'sinkexp'], writes=[dn[0]], inc=(c == 3))
                    cx.op('act', lambda e: e.activation(out=dn[1][:], in_=dn[1][:], func=AF.Ln), reads=[dn[0]], writes=[dn[0]])
                    cx.op('act', lambda e: e.activation(out=dn[1][:], in_=dn[1][:], func=AF.Exp, scale=-1.0), reads=[dn[0]], writes=[dn[0]])

                def s_fin2(qb):
                    q0 = 128 * qb
                    Oo = B[4 + qb % 2]
                    dn = dns[qb % 2]
                    for c, j in enumerate(JORD):
                        hp = 64 * (j % 2)
                        cx.op('dve', lambda e: e.tensor_tensor(out=mix[hp:hp + 64, 4 + 2 * hk + j // 2, q0:q0 + 128],
                                                                in0=Oo[1][hp:hp + 64, 128 * c:128 * c + 128], in1=dn[1][hp:hp + 64, 128 * c:128 * c + 128], op=ALU.mult),
                              reads=[Oo[0], dn[0]], writes=[('mix', 4 + 2 * hk + j // 2, qb)], inc=(c == 3))

                s_scores(0)
                for n, (qb, kb, idx, nk) in enumerate(units):
                    if idx == 0 and qb >= 2:
                        s_fin2(qb - 2)
                    if n + 1 < len(units):
                        s_scores(n + 1)
                    s_pv(n)
                    if idx == nk - 1 and qb >= 1:
                        s_fin1(qb - 1)
                s_fin2(15)
                s_fin1(16)
                s_fin2(16)
            cx.barrier()
        if DBG.get('a_cut', 99) <= 7:
            return
        with ExitStack() as ps:
            wo = sb("woA", [128, 8, D], BF16, ps)
            cx.dma('pool', 'w1', wo[:], w_out.rearrange("(k p) d -> p k d", p=128), writes=['wo'])
            it = 0
            for (p0, n) in TILES_ALL:
                for d in range(8):
                    pb = B[it % 4]
                    it += 1
                    for c in range(8):
                        cx.op('pe', lambda e: e.matmul(pb[1][:, :n], wo[:, c, d * 128:(d + 1) * 128], mix[:, c, p0:p0 + n], start=(c == 0), stop=(c == 7)),
                              reads=['wo'] + blk_keys('mix', c, p0, n), writes=[pb[0]], inc=(c == 7))
                    cx.op('dve', lambda e: e.tensor_tensor(out=g.h[:, d, p0:p0 + n], in0=pb[1][:, :n], in1=g.h[:, d, p0:p0 + n], op=ALU.add),
                          reads=[pb[0]] + blk_keys('h', d, p0, n), writes=blk_keys('h', d, p0, n))
            cx.barrier()


def emit_phase_c(cx, g, sb, w_pw1, w_pw2):
    nc = cx.nc
    w1v = w_pw1.rearrange("(k p) f -> p k f", p=128)
    with ExitStack() as pc:
        u = sb("uC", [128, 8, NPOS], BF16, pc)
        w2s = sb("w2C", [128, 8, D], BF16, pc)
        cx.op('pool', lambda e: e.memset(u[:, :, 0:128], 0.0), writes=[('u', c, 0) for c in range(8)])
        with ExitStack() as ps:
            xn = sb("xnC", [128, 8, NPOS], BF16, ps)
            sq = sb("sqC", [128, 8, 512], BF16, ps)
            rstd = [(('rstd', i), sb(f"rstdC{i}", [128, 512], F32, ps)) for i in range(2)]
            wa = [sb(f"waC{i}", [128, 8, 128], BF16, ps) for i in range(2)]
            wg = [sb(f"wgC{i}", [128, 8, 128], BF16, ps) for i in range(2)]
            sg = [(('sg', i), sb(f"sgC{i}", [128, 512], F32, ps)) for i in range(2)]
            def norm_c(ti):
                emit_rmsnorm(cx, g, V_CNORM, [TILES_ALL[ti]], xn, 'xn', sq, [rstd[ti % 2]], [g.psx[ti % 2]])
            n_it = 0
            for cc in range(8):
                s = cc % 2
                cx.dma('pool', f'w{s}', wa[s][:], w1v[:, :, cc * 128:(cc + 1) * 128], writes=[('wa', s)])
                cx.dma('pool', f'w{s}', wg[s][:], w1v[:, :, D + cc * 128:D + (cc + 1) * 128], writes=[('wg', s)])
                if cc == 0:
                    norm_c(0)
                if cc == 1:
                    cx.dma('pool', 'w1', w2s[:], w_pw2.rearrange("(k p) d -> p k d", p=128), writes=['w2C'])
                for ti_c, (p0, n) in enumerate(TILES_ALL):
                    if cc == 0 and ti_c + 1 < len(TILES_ALL):
                        norm_c(ti_c + 1)
                    pa = g.ps1[n_it % 2]
                    pg = g.ps3[n_it % 2]
                    sgb = sg[n_it % 2]
                    n_it += 1
                    for k in range(8):
                        cx.op('pe', lambda e: e.matmul(pa[1][:, :n], wa[s][:, k, :], xn[:, k, p0:p0 + n], start=(k == 0), stop=(k == 7)),
                              reads=[('wa', s)] + blk_keys('xn', k, p0, n), writes=[pa[0]], inc=(k == 7))
                    for k in range(8):
                        cx.op('pe', lambda e: e.matmul(pg[1][:, :n], wg[s][:, k, :], xn[:, k, p0:p0 + n], start=(k == 0), stop=(k == 7)),
                              reads=[('wg', s)] + blk_keys('xn', k, p0, n), writes=[pg[0]], inc=(k == 7))
                    cx.op('act', lambda e: e.activation(out=sgb[1][:, :n], in_=pg[1][:, :n], func=AF.Sigmoid),
                          reads=[pg[0]], writes=[sgb[0]])
                    if p0 == PAD:
                        cx.op('dve', lambda e: e.tensor_tensor(out=u[:, cc, p0:p0 + n], in0=pa[1][:, :n], in1=sgb[1][:, :n], op=ALU.mult),
                              reads=[pa[0], sgb[0], ('u', cc, 0)], writes=[('u', cc, 0)])
                    else:
                        cx.op('dve', lambda e: e.tensor_tensor(out=u[:, cc, p0:p0 + n], in0=pa[1][:, :n], in1=sgb[1][:, :n], op=ALU.mult),
                              reads=[pa[0], sgb[0]], writes=blk_keys('u', cc, p0, n))
            cx.barrier()
        with ExitStack() as ps:
            if DBG.get('c_cut') == 1:
                return
            dg = [sb(f"dgC{i}", [128, 31, 128], BF16, ps) for i in range(2)]
            y32s = [sb(f"y32C{i}", [128, 8, 512], F32, ps) for i in range(2)]
            ybf = sb("ybfC", [128, 8, 512], BF16, ps)
            ysq = sb("ysqC", [128, 8, 512], BF16, ps)
            mean = sb("meanC", [128, 512], F32, ps)
            msq = sb("msqC", [128, 512], F32, ps)
            rstd = sb("rstdC2", [128, 512], F32, ps)
            tt = [(('tt', i), sb(f"ttC{i}", [128, 512], F32, ps)) for i in range(2)]
            v = sb("vC", [128, 8, 512], BF16, ps)
            def c_conv_mm(ti, cc):
                p0, n = TILES_REAL[ti]
                s = (ti * 8 + cc) % 2
                py = g.ps1[(ti * 8 + cc) % 2]
                col = V_WDW + cc * 31
                cx.op('dve', lambda e: e.tensor_tensor(out=dg[s][:], in0=g.ident[:].unsqueeze(1).to_broadcast([128, 31, 128]),
                                                        in1=g.vecs[:, col:col + 31].unsqueeze(2).to_broadcast([128, 31, 128]), op=ALU.mult),
                      reads=[], writes=[('dg', s)])
                for j in range(31):
                    q0 = p0 - 30 + j
                    cx.op('pe', lambda e: e.matmul(py[1][:, :n], dg[s][:, j, :], u[:, cc, q0:q0 + n], start=(j == 0), stop=(j == 30)),
                          reads=[('dg', s)] + blk_keys('u', cc, p0 - 30, n + 30), writes=[py[0]], inc=(j == 30))

            def c_conv_ev(ti, cc):
                p0, n = TILES_REAL[ti]
                y32 = y32s[ti % 2]
                py = g.ps1[(ti * 8 + cc) % 2]
                bcol = V_CBDW + cc
                cx.op('act', lambda e: e.activation(out=y32[:, cc, :], in_=py[1][:, :n], func=AF.Identity, bias=g.vecs[:, bcol:bcol + 1], scale=1.0),
                      reads=[py[0]], writes=[('y32', ti % 2, cc)])
                cx.op('pool', lambda e: e.tensor_copy(out=ybf[:, cc, :], in_=y32[:, cc, :]), reads=[('y32', ti % 2, cc)], writes=[('ybf', cc)])
                cx.op('act', lambda e: e.activation(out=ysq[:, cc, :], in_=y32[:, cc, :], func=AF.Square), reads=[('y32', ti % 2, cc)], writes=[('ysq', cc)])

            def c_stats(ti):
                p0, n = TILES_REAL[ti]
                psm = g.psx[0]
                psq = g.psx[1]
                for cc in range(8):
                    cx.op('pe', lambda e: e.matmul(psm[1][:, :n], g.ones_bf[:], ybf[:, cc, :], start=(cc == 0), stop=(cc == 7)),
                          reads=[('ybf', cc)], writes=[psm[0]], inc=(cc == 7))
                for cc in range(8):
                    cx.op('pe', lambda e: e.matmul(psq[1][:, :n], g.ones_bf[:], ysq[:, cc, :], start=(cc == 0), stop=(cc == 7)),
                          reads=[('ysq', cc)], writes=[psq[0]], inc=(cc == 7))
                cx.op('act', lambda e: e.activation(out=mean[:], in_=psm[1][:], func=AF.Copy, scale=1.0 / D), reads=[psm[0]], writes=['mean'])
                cx.op('act', lambda e: e.activation(out=msq[:], in_=psm[1][:], func=AF.Square, scale=1.0 / D), reads=[psm[0]], writes=['msq'])
                cx.op('dve', lambda e: e.scalar_tensor_tensor(out=rstd[:], in0=psq[1][:], scalar=1.0 / D, in1=msq[:], op0=ALU.mult, op1=ALU.subtract),
                      reads=[psq[0], 'msq'], writes=['rstdC'])
                cx.op('act', lambda e: e.activation(out=rstd[:], in_=rstd[:], func=AF.Ln, bias=g.eps_ap, scale=1.0), reads=['rstdC'], writes=['rstdC'])
                cx.op('act', lambda e: e.activation(out=rstd[:], in_=rstd[:], func=AF.Exp, scale=-0.5), reads=['rstdC'], writes=['rstdC'])

            def c_norm(ti, cc):
                y32 = y32s[ti % 2]
                t = tt[cc % 2]
                cx.op('dve', lambda e: e.tensor_tensor(out=t[1][:], in0=y32[:, cc, :], in1=mean[:], op=ALU.subtract),
                      reads=[('y32', ti % 2, cc), 'mean'], writes=[t[0]])
                cx.op('pool', lambda e: e.tensor_tensor(out=t[1][:], in0=t[1][:], in1=rstd[:], op=ALU.mult),
                      reads=[t[0], 'rstdC'], writes=[t[0]])
                gc, bc = V_CLNG + cc, V_CLNB + cc
                cx.op('dve', lambda e: e.tensor_scalar(out=t[1][:], in0=t[1][:], scalar1=g.vecs[:, gc:gc + 1], scalar2=g.vecs[:, bc:bc + 1],
                                                        op0=ALU.mult, op1=ALU.add),
                      reads=[t[0]], writes=[t[0]])
                cx.op('act', lambda e: e.activation(out=v[:, cc, :], in_=t[1][:], func=AF.Silu),
                      reads=[t[0]], writes=[('v', cc)])

            def c_pw2(ti):
                p0, n = TILES_REAL[ti]
                for d in range(8):
                    pb = g.pso[d % 2]
                    for cc in range(8):
                        cx.op('pe', lambda e: e.matmul(pb[1][:, :n], w2s[:, cc, d * 128:(d + 1) * 128], v[:, cc, :], start=(cc == 0), stop=(cc == 7)),
                              reads=['w2C', ('v', cc)], writes=[pb[0]], inc=(cc == 7))
                    cx.op('dve', lambda e: e.tensor_tensor(out=g.h[:, d, p0:p0 + n], in0=pb[1][:, :n], in1=g.h[:, d, p0:p0 + n], op=ALU.add),
                          reads=[pb[0]] + blk_keys('h', d, p0, n), writes=blk_keys('h', d, p0, n))

            for cc in range(8):
                c_conv_mm(0, cc)
                c_conv_ev(0, cc)
            c_stats(0)
            for ti in range(4):
                for cc in range(8):
                    if ti + 1 < 4:
                        c_conv_mm(ti + 1, cc)
                    c_norm(ti, cc)
                    if ti + 1 < 4:
                        c_conv_ev(ti + 1, cc)
                c_pw2(ti)
                if ti + 1 < 4:
                    c_stats(ti + 1)
            cx.barrier()


def emit_phase_d(cx, g, sb, w_router, m_w1, m_w3, m_w2):
    nc = cx.nc
    with ExitStack() as po:
        xn = sb("xnD", [128, 8, NPOS], BF16, po)
        gates = sb("gatesD", [128, 16, NE], F32, po)
        sq = sb("sqD", [128, 4, 512], BF16, po)
        rstd = [(('rstd', i), sb(f"rstdD{i}", [128, 512], F32, po)) for i in range(1)]
        x32 = (('x32', 0), sb("x32D0", [128, 8, 512], F32, po))
        wr = sb("wrD", [128, 8, NE], F32, po)
        lg = sb("lgD", [128, 4 * NE], F32, po)
        mx8 = sb("mx8D", [128, 4, 8], F32, po)
        ex = sb("exD", [128, 4 * NE], F32, po)
        msk = sb("mskD", [128, 4 * NE], F32, po)
        sm = sb("smD", [128, 4, 4], F32, po)
        dgt = [(('dgt', i), sb(f"dgtD{i}", [128, 128], F32, po)) for i in range(2)]
        Gt = [(('G', i), sb(f"GD{i}", [128, SEQ], F32, po)) for i in range(2)]
        g.w1g = [sb(f"w1gD{i}", [128, 8, 512], BF16, po) for i in range(2)]
        g.w3g = [sb(f"w3gD{i}", [128, 8, 512], BF16, po) for i in range(2)]
        g.w2g = [sb(f"w2gD{i}", [128, 4, 1024], BF16, po) for i in range(2)]
        g.hh = [sb(f"hhD{i}", [128, 4, 512], BF16, po) for i in range(2)]
        g.silu = [(('silu', i), sb(f"siluD{i}", [128, 512], F32, po)) for i in range(3)]

        groups = []
        for e in range(NE):
            groups += ffn_groups(m_w1[e], m_w3[e], m_w2[e], gate=Gt[e % 2])
        slot0 = ffn_load(cx, g, groups[0])
        cx.dma('sp', 'io', wr[:], w_router.rearrange("(k p) e -> p k e", p=128), writes=['wr'])

        def router(ti):
            xb = x32
            pl = g.psx[1]
            for b in range(4):
                for k in range(8):
                    cx.op('pe', lambda e: e.matmul(pl[1][:, b * NE:(b + 1) * NE], xb[1][:, k, b * 128:(b + 1) * 128], wr[:, k, :], start=(k == 0), stop=(k == 7)),
                          reads=[(xb[0], k), 'wr'], writes=[pl[0]], inc=(k == 7))
            lg3 = lg[:].rearrange("p (b e) -> p b e", b=4)
            ex3 = ex[:].rearrange("p (b e) -> p b e", b=4)
            msk3 = msk[:].rearrange("p (b e) -> p b e", b=4)
            cx.op('dve', lambda e: e.tensor_copy(out=lg[:], in_=pl[1][:, :4 * NE]), reads=[pl[0]], writes=['lg'])
            for b in range(4):
                cx.op('dve', lambda e: e.max(out=mx8[:, b, :], in_=lg3[:, b, :]), reads=['lg'], writes=['mx8'], inc=(b == 3))
            m1 = mx8[:, :, 0:1]
            m2 = mx8[:, :, 1:2]
            cx.op('dve', lambda e: e.tensor_tensor(out=ex3, in0=lg3, in1=m1.to_broadcast([128, 4, NE]), op=ALU.subtract),
                  reads=['lg', 'mx8'], writes=['ex'])
            cx.op('act', lambda e: e.activation(out=ex[:], in_=ex[:], func=AF.Exp), reads=['ex'], writes=['ex'])
            cx.op('dve', lambda e: e.tensor_tensor(out=sm[:, :, 0:1], in0=m2, in1=m1, op=ALU.subtract), reads=['mx8'], writes=['sm0'])
            cx.op('act', lambda e: e.activation(out=sm[:, :, 1:2], in_=sm[:, :, 0:1], func=AF.Exp), reads=['sm0'], writes=['sm1'])
            cx.op('dve', lambda e: e.tensor_scalar(out=sm[:, :, 2:3], in0=sm[:, :, 1:2], scalar1=1.0, scalar2=None, op0=ALU.add),
                  reads=['sm1'], writes=['sm2'])
            cx.op('dve', lambda e: e.reciprocal(out=sm[:, :, 3:4], in_=sm[:, :, 2:3]), reads=['sm2'], writes=['sm3'])
            cx.op('dve', lambda e: e.tensor_tensor(out=msk3, in0=lg3, in1=m2.to_broadcast([128, 4, NE]), op=ALU.is_ge),
                  reads=['lg', 'mx8'], writes=['msk'])
            cx.op('dve', lambda e: e.tensor_tensor(out=ex3, in0=ex3, in1=sm[:, :, 3:4].to_broadcast([128, 4, NE]), op=ALU.mult),
                  reads=['ex', 'sm3'], writes=['ex'])
            cx.op('dve', lambda e: e.tensor_tensor(out=gates[:, ti * 4:(ti + 1) * 4, :], in0=ex3, in1=msk3, op=ALU.mult),
                  reads=['ex', 'msk'], writes=[('gates', ti * 4 + b) for b in range(4)])

        def build_gate(e, ti):
            Gb = Gt[e % 2]
            pg = g.psx[1]
            for b in range(4):
                blk = ti * 4 + b
                db = dgt[blk % 2]
                cx.op('dve', lambda en: en.tensor_scalar(out=db[1][:], in0=g.ident[:], scalar1=gates[:, blk, e:e + 1], scalar2=None, op0=ALU.mult),
                      reads=[('gates', blk), 'ident'], writes=[db[0]])
                cx.op('pe', lambda en: en.matmul(pg[1][:, b * 128:(b + 1) * 128], g.ones_f[:], db[1][:], start=True, stop=True),
                      reads=[db[0]], writes=[pg[0]], inc=True)
            cx.op('act', lambda en: en.activation(out=Gb[1][:, ti * 512:(ti + 1) * 512], in_=pg[1][:], func=AF.Copy),
                  reads=[pg[0]], writes=[Gb[0] + (ti,)])

        def stage1(ti):
            emit_rmsnorm(cx, g, V_MNORM, [TILES_REAL[ti]], xn, 'xn', sq, [rstd[0]], [g.psx[1]], xn32=lambda _t: x32, nsq=4)
            router(ti)
            build_gate(0, ti)

        def pre_step(gi, ti):
            if gi == 0 and ti + 1 < 4:
                stage1(ti + 1)

        def on_group(gi):
            e, j = divmod(gi, 7)
            if j == 2 and e + 1 < NE:
                for ti in range(4):
                    build_gate(e + 1, ti)

        stage1(0)
        emit_ffn(cx, g, groups, TILES_REAL, xn, on_group=on_group, pre_step=pre_step, first_slot=slot0)
        cx.barrier()


_CACHE = {}


def _host_inputs(inputs, b):
    x = np.asarray(inputs['x'], dtype=np.float32)
    xT = np.zeros((D, NPOS), np.float32)
    xT[:, PAD:PAD + NMETA] = np.asarray(inputs['meta'], np.float32).T
    xT[:, PAD + NMETA:] = x[b].T
    return xT


def _vecs(inputs):
    def fm(v):
        return np.asarray(v, np.float32).reshape(8, 128).T
    vecs = np.zeros((128, NV), np.float32)
    vecs[:, V_ANORM:V_ANORM + 8] = fm(inputs['a_norm'][0])
    vecs[:, V_FNORM:V_FNORM + 8] = fm(inputs['f_norm'][0])
    vecs[:, V_CNORM:V_CNORM + 8] = fm(inputs['c_norm'][0])
    vecs[:, V_MNORM:V_MNORM + 8] = fm(inputs['m_norm'][0])
    vecs[:, V_CBDW:V_CBDW + 8] = fm(inputs['c_b_dw'][0])
    vecs[:, V_CLNG:V_CLNG + 8] = fm(inputs['c_ln_g'][0])
    vecs[:, V_CLNB:V_CLNB + 8] = fm(inputs['c_ln_b'][0])
    wdw = np.asarray(inputs['c_w_dw'][0], np.float32)
    vecs[:, V_WDW:V_WDW + 248] = wdw.reshape(31, 8, 128).transpose(2, 1, 0).reshape(128, 8 * 31)
    vecs[:, V_QN] = np.tile(np.asarray(inputs['a_q_norm'][0], np.float32), 2)
    vecs[:, V_KN] = np.tile(np.asarray(inputs['a_k_norm'][0], np.float32), 2)
    vecs[:, V_ON] = np.asarray(inputs['a_o_norm'][0], np.float32)
    vecs[:, V_SINK:V_SINK + 8] = np.asarray(inputs['a_sinks'][0], np.float32)[None, :]
    return vecs


def _consts():
    c = np.zeros((128, NC), np.float32)
    i = np.arange(128)
    R = np.zeros((128, 128), np.float32)
    for m in range(128):
        d = m % 64
        if d < 32:
            R[m + 32, m] = -1.0
        else:
            R[m - 32, m] = 1.0
    c[:, C_ROT:C_ROT + 128] = R
    same_half = (i[:, None] // 64) == (i[None, :] // 64)
    tri = (i[:, None] <= i[None, :]) & same_half
    c[:, C_MC:C_MC + 128] = tri.astype(np.float32) * (-1.0 / 16.0)
    c[:, C_GMASK:C_GMASK + 128] = tri.astype(np.float32)
    tri2 = (i[:, None] > i[None, :]) & same_half
    c[:, C_MC2:C_MC2 + 128] = tri2.astype(np.float32) * (-1.0 / 16.0)
    j = i[:, None]
    q = i[None, :]
    m_same = (j <= q)
    m_prev = (j > q)
    valid0 = (j >= PAD)
    for t, m in enumerate([m_same, m_prev, m_same & valid0, m_prev & valid0]):
        c[:, C_SMASK + 512 * t:C_SMASK + 512 * (t + 1)] = np.tile(np.broadcast_to(m, (128, 128)).astype(np.float32), (1, 4))
    inv_freq = 1.0 / (10000.0 ** (np.arange(0, 64, 2, dtype=np.float32) / 64.0))
    pos = (np.arange(NPOS, dtype=np.float32) - PAD)
    ang = pos[None, :] * inv_freq[(i % 64) % 32][:, None]
    c[:, C_COS:C_COS + NPOS] = np.cos(ang)
    c[:, C_SIN:C_SIN + NPOS] = np.sin(ang)
    return c


def make_in_maps(inputs, xTs, names=None):
    shared = {
        "vecs": _vecs(inputs),
        "ident": np.eye(128, dtype=np.float32),
        "consts": _consts(),
        "a_w_in": np.ascontiguousarray(inputs['a_w_in'][0]), "a_w_gate2": np.ascontiguousarray(inputs['a_w_gate2'][0]),
        "a_bgate": np.ascontiguousarray(np.asarray(inputs['a_b_gate'][0], np.float32).reshape(1, 256)),
        "a_w_out": np.ascontiguousarray(inputs['a_w_out'][0]),
        "f_w1": np.ascontiguousarray(inputs['f_w1'][0]), "f_w3": np.ascontiguousarray(inputs['f_w3'][0]),
        "f_w2": np.ascontiguousarray(inputs['f_w2'][0]),
        "c_w_pw1": np.ascontiguousarray(inputs['c_w_pw1'][0]), "c_w_pw2": np.ascontiguousarray(inputs['c_w_pw2'][0]),
        "m_w_router": np.ascontiguousarray(inputs['m_w_router'][0]),
        "m_w1": np.ascontiguousarray(inputs['m_w1'][0]), "m_w3": np.ascontiguousarray(inputs['m_w3'][0]),
        "m_w2": np.ascontiguousarray(inputs['m_w2'][0]),
    }
    if names is not None:
        shared = {k: v for k, v in shared.items() if k in names}
    return [dict(shared, xT=xTs[b]) for b in range(8)]


def kernel(**inputs):
    if 'full' not in _CACHE:
        _CACHE['full'] = build_program("ABCD", debug_out=False)[0]
    nc = _CACHE['full']
    xTs = [_host_inputs(inputs, b) for b in range(8)]
    in_maps = make_in_maps(inputs, xTs)
    res = run_bass_kernel_spmd(nc, in_maps, core_ids=list(range(8)))
    out = np.stack([np.ascontiguousarray(r["outT"].T) for r in res.results], axis=0)
    return out.astype(np.float32)
```

```python
import numpy as np
from contextlib import ExitStack
import concourse.bass as bass
import concourse.mybir as mybir
from concourse.bass_utils import run_bass_kernel_spmd

F32 = mybir.dt.float32
BF16 = mybir.dt.bfloat16
AF = mybir.ActivationFunctionType
ALU = mybir.AluOpType

D = 1024
NPOS = 2176
PAD = 112
NMETA = 16
SEQ = 2048
DFF = 3584
NE = 8
EPS = 1e-6
TILES_ALL = [(112, 16), (128, 512), (640, 512), (1152, 512), (1664, 512)]
TILES_REAL = TILES_ALL[1:]

V_ANORM, V_FNORM, V_CNORM, V_MNORM, V_CBDW, V_CLNG, V_CLNB = 0, 8, 16, 24, 32, 40, 48
V_WDW = 56
V_QN = V_WDW + 8 * 31
V_KN = V_QN + 1
V_ON = V_KN + 1
V_SINK = V_ON + 1
NV = V_SINK + 8
C_ROT, C_MC, C_GMASK, C_SMASK = 0, 128, 256, 384
C_COS = C_SMASK + 4 * 512
C_SIN = C_COS + NPOS
C_MC2 = C_SIN + NPOS
NC = C_MC2 + 128
JORD = [0, 2, 1, 3]
TILES_BLK = [(0, 128), (128, 512), (640, 512), (1152, 512), (1664, 512)]
O_GQ, O_GK, O_GV, O_GR, O_GLR, O_SQ, O_SK, O_SV = 0, 256, 512, 1024, 1536, 1552, 2064, 2192


class Ctx:
    ENG = ['pe', 'act', 'dve', 'pool', 'sp']

    def __init__(self, nc, es):
        self.nc = nc
        self.es = es
        self.eng = dict(pe=nc.tensor, act=nc.scalar, dve=nc.vector, pool=nc.gpsimd, sp=nc.sync)
        self.sem = {e: es.enter_context(nc.semaphore('c_' + e)) for e in self.ENG}
        self.cnt = {e: 0 for e in self.ENG}
        self.seen = {e: {} for e in self.ENG}
        self.lw = {}
        self.rd = {}
        self.dsem = {}
        self.dtot = {}
        self.nwait = 0
        self.ninst = 0

    def stream(self, name):
        if name not in self.dsem:
            self.dsem[name] = self.es.enter_context(self.nc.semaphore('d_' + name))
            self.dtot[name] = 0
        return name

    def _semval(self, tgt, val):
        if isinstance(tgt, tuple):
            return self.dsem[tgt[1]], max(val, self.dtot[tgt[1]])
        return self.sem[tgt], val

    def _waits(self, E, reads, writes):
        need = {}

        def add(tgt, val):
            if need.get(tgt, 0) < val:
                need[tgt] = val
        for k in reads:
            w = self.lw.get(k)
            if w is not None and not (w[0] == E and E == 'pe'):
                add(*w)
        for k in writes:
            w = self.lw.get(k)
            if w is not None and not (w[0] == E and E == 'pe'):
                add(*w)
            for t, v in self.rd.get(k, {}).items():
                if not (t == E and E == 'pe'):
                    add(t, v)
        for tgt, val in need.items():
            if tgt == E and val > self.cnt[E]:
                continue
            sem, val = self._semval(tgt, val)
            if self.seen[E].get(tgt, 0) >= val:
                continue
            self.eng[E].wait_ge(sem, val)
            self.seen[E][tgt] = val
            self.nwait += 1

    def op(self, E, fn, reads=(), writes=(), inc=True):
        self._waits(E, reads, writes)
        ins = fn(self.eng[E])
        idx = self.cnt[E] + 1
        if inc:
            ins.then_inc(self.sem[E], 1)
            self.cnt[E] = idx
        for k in writes:
            self.lw[k] = (E, idx)
            self.rd[k] = {}
        for k in reads:
            self.rd.setdefault(k, {})[E] = idx
        self.ninst += 1
        return ins

    def dma(self, Q, stream, out, in_, reads=(), writes=()):
        self.stream(stream)
        self._waits(Q, reads, writes)
        ins = self.eng[Q].dma_start(out=out, in_=in_)
        self.dtot[stream] += 16
        ins.then_inc(self.dsem[stream], 16)
        tgt = ('dma', stream)
        for k in writes:
            self.lw[k] = (tgt, self.dtot[stream])
            self.rd[k] = {}
        for k in reads:
            self.rd.setdefault(k, {})[tgt] = self.dtot[stream]
        return ins

    def barrier(self):
        for E in self.ENG:
            for F in self.ENG:
                if not (F == E and E in ('pe', 'sp')) and self.cnt[F] > self.seen[E].get(F, 0):
                    self.eng[E].wait_ge(self.sem[F], self.cnt[F])
                    self.seen[E][F] = self.cnt[F]
            for s in self.dsem:
                tgt = ('dma', s)
                if self.dtot[s] > self.seen[E].get(tgt, 0):
                    self.eng[E].wait_ge(self.dsem[s], self.dtot[s])
                    self.seen[E][tgt] = self.dtot[s]
        self.lw.clear()
        self.rd.clear()


def blk_keys(name, c, p0, n):
    return [(name, c, b) for b in range(p0 // 128, (p0 + n - 1) // 128 + 1)]


DBG = {}


class G:
    pass


def emit_rmsnorm(cx, g, vcol, tiles, xn, xn_name, sq, rstd_t, psn, xn32=None, nsq=8):
    nc = cx.nc
    for ti, (p0, n) in enumerate(tiles):
        pb = psn[ti % len(psn)]
        for c0 in range(0, 8, nsq):
            for c in range(c0, c0 + nsq):
                cx.op('act', lambda e: e.activation(out=sq[:, c % nsq, :n], in_=g.h[:, c, p0:p0 + n], func=AF.Square),
                      reads=blk_keys('h', c, p0, n), writes=[('sq', c % nsq)])
            for c in range(c0, c0 + nsq):
                cx.op('pe', lambda e: e.matmul(pb[1][:, :n], g.ones_bf[:], sq[:, c % nsq, :n], start=(c == 0), stop=(c == 7)),
                      reads=[('sq', c % nsq)], writes=[pb[0]], inc=True)
        rt = rstd_t[ti % 2]
        cx.op('act', lambda e: e.activation(out=rt[1][:, :n], in_=pb[1][:, :n], func=AF.Ln, bias=g.eps_ap, scale=1.0 / D),
              reads=[pb[0]], writes=[rt[0]])
        cx.op('act', lambda e: e.activation(out=rt[1][:, :n], in_=rt[1][:, :n], func=AF.Exp, scale=-0.5), reads=[rt[0]], writes=[rt[0]])
        for c in range(8):
            if xn32 is None:
                cx.op('dve', lambda e: e.scalar_tensor_tensor(out=xn[:, c, p0:p0 + n], in0=g.h[:, c, p0:p0 + n],
                                                               scalar=g.vecs[:, vcol + c:vcol + c + 1], in1=rt[1][:, :n],
                                                               op0=ALU.mult, op1=ALU.mult),
                      reads=blk_keys('h', c, p0, n) + [rt[0]], writes=blk_keys(xn_name, c, p0, n))
            else:
                x32 = xn32(ti)
                cx.op('dve', lambda e: e.scalar_tensor_tensor(out=x32[1][:, c, :n], in0=g.h[:, c, p0:p0 + n],
                                                               scalar=g.vecs[:, vcol + c:vcol + c + 1], in1=rt[1][:, :n],
                                                               op0=ALU.mult, op1=ALU.mult),
                      reads=blk_keys('h', c, p0, n) + [rt[0]], writes=[(x32[0], c)])
                cx.op('pool', lambda e: e.tensor_copy(out=xn[:, c, p0:p0 + n], in_=x32[1][:, c, :n]),
                      reads=[(x32[0], c)], writes=blk_keys(xn_name, c, p0, n))


def ffn_groups(w1, w3, w2, gate=None):
    w1v = w1.rearrange("(k p) f -> p k f", p=128)
    w3v = w3.rearrange("(k p) f -> p k f", p=128)
    return [dict(w1=w1v[:, :, j * 512:(j + 1) * 512], w3=w3v[:, :, j * 512:(j + 1) * 512],
                 w2=w2[j * 512:(j + 1) * 512, :].rearrange("(m p) d -> p m d", p=128), gate=gate)
            for j in range(DFF // 512)]


def ffn_load(cx, g, gr):
    s = g.wslot_n % 2
    g.wslot_n += 1
    cx.dma('pool', f'w{s}', g.w1g[s][:], gr['w1'], writes=[('w1g', s)])
    cx.dma('pool', f'w{s}', g.w3g[s][:], gr['w3'], writes=[('w3g', s)])
    cx.dma('pool', f'w{s}', g.w2g[s][:], gr['w2'], writes=[('w2g', s)])
    return s


def emit_ffn(cx, g, groups, tiles, xn, on_group=None, pre_step=None, first_slot=None):
    NG = len(groups)
    NT = len(tiles)

    def load(gi):
        return ffn_load(cx, g, groups[gi])

    slots = {}
    pending = None

    def down(gi, ti, hb):
        s = slots[gi]
        p0, n = tiles[ti]
        for d in range(8):
            pb = g.pso3[g.pso_n % 3]
            g.pso_n += 1
            for m in range(4):
                cx.op('pe', lambda e: e.matmul(pb[1][:, :n], g.w2g[s][:, m, d * 128:(d + 1) * 128], g.hh[hb][:, m, :n],
                                               start=(m == 0), stop=(m == 3)),
                      reads=[('w2g', s), ('hh', hb, m)], writes=[pb[0]], inc=(m == 3))
            cx.op('dve', lambda e: e.tensor_tensor(out=g.h[:, d, p0:p0 + n], in0=pb[1][:, :n], in1=g.h[:, d, p0:p0 + n], op=ALU.add),
                  reads=[pb[0]] + blk_keys('h', d, p0, n), writes=blk_keys('h', d, p0, n))

    slots[0] = first_slot if first_slot is not None else load(0)
    it = 0
    for gi in range(NG):
        gate = groups[gi]['gate']
        for ti in range(NT):
            s = slots[gi]
            p0, n = tiles[ti]
            hb = it % 2
            it += 1
            for m in range(4):
                p1 = g.ps1[g.ps1_n % 2]
                p3 = g.ps3[g.ps1_n % 2]
                g.ps1_n += 1
                for k in range(8):
                    cx.op('pe', lambda e: e.matmul(p1[1][:, :n], g.w1g[s][:, k, m * 128:(m + 1) * 128], xn[:, k, p0:p0 + n],
                                                   start=(k == 0), stop=(k == 7)),
                          reads=[('w1g', s)] + blk_keys('xn', k, p0, n), writes=[p1[0]], inc=(k == 7))
                for k in range(8):
                    cx.op('pe', lambda e: e.matmul(p3[1][:, :n], g.w3g[s][:, k, m * 128:(m + 1) * 128], xn[:, k, p0:p0 + n],
                                                   start=(k == 0), stop=(k == 7)),
                          reads=[('w3g', s)] + blk_keys('xn', k, p0, n), writes=[p3[0]], inc=(k == 7))
                sb = g.silu[g.silu_n % len(g.silu)]
                g.silu_n += 1
                cx.op('act', lambda e: e.activation(out=sb[1][:, :n], in_=p1[1][:, :n], func=AF.Silu),
                      reads=[p1[0]], writes=[sb[0]])
                if gate is not None:
                    cx.op('dve', lambda e: e.tensor_tensor(out=sb[1][:, :n], in0=p3[1][:, :n], in1=sb[1][:, :n], op=ALU.mult),
                          reads=[p3[0], sb[0]], writes=[sb[0]])
                    cx.op('pool', lambda e: e.tensor_tensor(out=g.hh[hb][:, m, :n], in0=sb[1][:, :n], in1=gate[1][:, p0 - 128:p0 - 128 + n], op=ALU.mult),
                          reads=[sb[0], gate[0] + (ti,)], writes=[('hh', hb, m)])
                else:
                    cx.op('dve', lambda e: e.tensor_tensor(out=g.hh[hb][:, m, :n], in0=p3[1][:, :n], in1=sb[1][:, :n], op=ALU.mult),
                          reads=[p3[0], sb[0]], writes=[('hh', hb, m)])
            if pending is not None:
                down(*pending)
            pending = (gi, ti, hb)
            if ti == 0 and gi + 1 < NG:
                slots[gi + 1] = load(gi + 1)
            if pre_step is not None:
                pre_step(gi, ti)
        if on_group is not None:
            on_group(gi)
    down(*pending)


def build_program(phases="ABCD", debug_out=False):
    nc = bass.Bass("TRN2", target_bir_lowering=False)
    dt_in = {}

    def din(name, shape):
        dt_in[name] = nc.dram_tensor(name, list(shape), F32, kind="ExternalInput").ap()
        return dt_in[name]

    xT = din("xT", [D, NPOS])
    vecs_d = din("vecs", [128, NV])
    ident_d = din("ident", [128, 128])
    if 'A' in phases:
        consts_d = din("consts", [128, NC])
        a_w_in = din("a_w_in", [D, 2320]); a_w_gate2 = din("a_w_gate2", [16, 256]); a_bgate = din("a_bgate", [1, 256])
        a_w_out = din("a_w_out", [D, D])
    if 'B' in phases:
        f_w1 = din("f_w1", [D, DFF]); f_w3 = din("f_w3", [D, DFF]); f_w2 = din("f_w2", [DFF, D])
    if 'C' in phases:
        c_w_pw1 = din("c_w_pw1", [D, 2 * D]); c_w_pw2 = din("c_w_pw2", [D, D])
    if 'D' in phases:
        m_w_router = din("m_w_router", [D, NE])
        m_w1 = din("m_w1", [NE, D, DFF]); m_w3 = din("m_w3", [NE, D, DFF]); m_w2 = din("m_w2", [NE, DFF, D])
    if debug_out:
        outT = nc.dram_tensor("outT", [D, NPOS], F32, kind="ExternalOutput").ap()
    else:
        outT = nc.dram_tensor("outT", [D, SEQ], F32, kind="ExternalOutput").ap()

    es = ExitStack()
    with es:
        cx = Ctx(nc, es)
        g = G()

        def sb(name, shape, dt, stack=es):
            return stack.enter_context(nc.sbuf_tensor(name, list(shape), dt))

        g.h = sb("h", [128, 8, NPOS], F32)
        g.vecs = sb("vecs_sb", [128, NV], F32)
        g.ident = sb("ident_sb", [128, 128], F32)
        g.ones_bf = sb("ones_bf", [128, 128], BF16)
        g.ones_f = sb("ones_f", [128, 128], F32)
        g.eps_t = sb("eps_t", [128, 1], F32)
        g.eps_ap = g.eps_t[:, 0:1]
        g.one_t = sb("one_t", [128, 1], F32)
        g.one_ap = g.one_t[:, 0:1]
        banks = [es.enter_context(nc.psum_tensor(f"psb{i}", [128, 512], F32)) for i in range(8)]
        g.ps1 = [(('ps', 0), banks[0]), (('ps', 1), banks[1])]
        g.ps3 = [(('ps', 2), banks[2]), (('ps', 3), banks[3])]
        g.pso = [(('ps', 4), banks[4]), (('ps', 5), banks[5])]
        g.psx = [(('ps', 6), banks[6]), (('ps', 7), banks[7])]
        g.pso3 = [g.pso[0], g.pso[1], g.psx[0]]
        g.ps1_n = g.pso_n = g.silu_n = g.wslot_n = 0

        hv = xT.rearrange("(c p) t -> p c t", p=128)
        cx.dma('sp', 'io', g.vecs[:], vecs_d, writes=['vecs'])
        cx.dma('sp', 'io', g.ident[:], ident_d, writes=['ident'])
        cx.op('dve', lambda e: e.memset(g.ones_bf[:], 1.0), writes=['ones_bf'])
        cx.op('dve', lambda e: e.memset(g.ones_f[:], 1.0), writes=['ones_f'])
        cx.op('dve', lambda e: e.memset(g.eps_t[:], EPS), writes=['eps'])
        cx.op('dve', lambda e: e.memset(g.one_t[:], 1.0), writes=['one'])
        cx.barrier()
        for ti, (p0, n) in enumerate(TILES_BLK):
            for c in range(8):
                cx.dma('sp', f'h{ti}', g.h[:, c, p0:p0 + n], hv[:, c, p0:p0 + n], writes=blk_keys('h', c, p0, n))

        if 'A' in phases:
            emit_phase_a(cx, g, sb, a_w_in, a_w_gate2, a_bgate, a_w_out, consts_d)

        if 'B' in phases:
            with ExitStack() as ps:
                xn = sb("xnB", [128, 8, NPOS], BF16, ps)
                sq = sb("sqB", [128, 8, 512], BF16, ps)
                rstd = [(('rstd', i), sb(f"rstdB{i}", [128, 512], F32, ps)) for i in range(2)]
                g.w1g = [sb(f"w1g{i}", [128, 8, 512], BF16, ps) for i in range(2)]
                g.w3g = [sb(f"w3g{i}", [128, 8, 512], BF16, ps) for i in range(2)]
                g.w2g = [sb(f"w2g{i}", [128, 4, 1024], BF16, ps) for i in range(2)]
                g.hh = [sb(f"hh{i}", [128, 4, 512], BF16, ps) for i in range(2)]
                g.silu = [(('silu', i), sb(f"silu{i}", [128, 512], F32, ps)) for i in range(2)]
                groups_b = ffn_groups(f_w1, f_w3, f_w2)
                slot0 = ffn_load(cx, g, groups_b[0])

                def norm_b(ti):
                    emit_rmsnorm(cx, g, V_FNORM, [TILES_ALL[ti]], xn, 'xn', sq, [rstd[ti % 2]], [g.psx[1]])

                def pre_b(gi, ti):
                    if gi == 0 and ti + 1 < len(TILES_ALL):
                        norm_b(ti + 1)
                norm_b(0)
                emit_ffn(cx, g, groups_b, TILES_ALL, xn, pre_step=pre_b, first_slot=slot0)
                cx.barrier()

        if 'C' in phases:
            emit_phase_c(cx, g, sb, c_w_pw1, c_w_pw2)

        if 'D' in phases:
            emit_phase_d(cx, g, sb, m_w_router, m_w1, m_w3, m_w2)

        ov = outT.rearrange("(c p) t -> p c t", p=128)
        for c in range(8):
            if debug_out:
                cx.dma('sp', 'io', ov[:, c, :], g.h[:, c, :], reads=[('h', c, b) for b in range(17)])
            else:
                cx.dma('sp', 'io', ov[:, c, :], g.h[:, c, 128:NPOS], reads=[('h', c, b) for b in range(17)])
        cx.barrier()
        g.stats = (cx.ninst, cx.nwait)
        g.in_names = list(dt_in.keys())
    return nc, g


def emit_phase_a(cx, g, sb, w_in, w_gate2, bgate, w_out, consts):
    nc = cx.nc
    wv = w_in.rearrange("(k p) f -> p k f", p=128)
    B = [g.ps1[0], g.ps1[1], g.ps3[0], g.ps3[1], g.pso[0], g.pso[1], g.psx[0], g.psx[1]]
    with ExitStack() as pa:
        xn = sb("xnA", [128, 8, NPOS], BF16, pa)
        mix = sb("mixA", [128, 8, NPOS], BF16, pa)
        with ExitStack() as p01:
            wglr = sb("wglr", [128, 8, 16], BF16, p01)
            wg2 = sb("wg2", [16, 256], F32, p01)
            bg = sb("bgA", [1, 256], F32, p01)
            mc = sb("mcA", [128, 128], F32, p01)
            gmask = sb("gmaskA", [128, 128], F32, p01)
            mc2 = sb("mc2A", [128, 128], F32, p01)
            wq = sb("wqA", [128, 8, 128], BF16, p01)
            wk = sb("wkA", [128, 8, 128], BF16, p01)
            wvv = sb("wvA", [128, 8, 256], BF16, p01)
            cx.dma('pool', 'w0', wglr[:], wv[:, :, O_GLR:O_GLR + 16], writes=['wglr'])
            cx.dma('sp', 'io', wg2[:], w_gate2, writes=['wg2'])
            cx.dma('sp', 'io', bg[:], bgate, writes=['bg'])
            cx.dma('sp', 'io', mc[:], consts[:, C_MC:C_MC + 128], writes=['mc'])
            cx.dma('sp', 'io', gmask[:], consts[:, C_GMASK:C_GMASK + 128], writes=['gmask'])
            cx.dma('sp', 'io', mc2[:], consts[:, C_MC2:C_MC2 + 128], writes=['mc2'])
            cx.dma('pool', 'w1', wq[:], wv[:, :, O_GQ:O_GQ + 128], writes=['wq'])
            cx.dma('pool', 'w1', wk[:], wv[:, :, O_GK:O_GK + 128], writes=['wk'])
            cx.dma('pool', 'w1', wvv[:], wv[:, :, O_GV:O_GV + 256], writes=['wvv'])
            with ExitStack() as ps:
                sq = sb("sqA", [128, 8, 512], BF16, ps)
                rstd = [(('rstd', i), sb(f"rstdA{i}", [128, 512], F32, ps)) for i in range(2)]
                cx.op('pool', lambda e: e.memset(xn[:, :, 0:PAD], 0.0), writes=[('xn', c, 0) for c in range(8)])
                emit_rmsnorm(cx, g, V_ANORM, TILES_ALL, xn, 'xn', sq, rstd, g.psx)
                cx.barrier()
            if DBG.get('a_cut', 99) <= 0:
                return
            with ExitStack() as ps:
                glrT = sb("glrT", [16, NPOS], F32, ps)
                ebT = sb("ebT", [128, NPOS], F32, ps)
                qtT = sb("qtT", [128, NPOS], BF16, ps)
                ktT = sb("ktT", [128, NPOS], BF16, ps)
                kttok = sb("kttok", [128, 17, 128], BF16, ps)
                vtok = sb("vtok", [128, 17, 256], BF16, ps)
                srT = sb("srT", [128, 2, NPOS], BF16, ps)
                wr = wvv
                sp = [(('sp', i), sb(f"spA{i}", [128, 128], F32, ps)) for i in range(2)]
                e1 = [(('e1', i), sb(f"e1A{i}", [128, 128], F32, ps)) for i in range(2)]
                emb = [(('emb', i), sb(f"embA{i}", [128, 128], F32, ps)) for i in range(2)]
                rec = [(('rec', i), sb(f"recA{i}", [128, 512], F32, ps)) for i in range(2)]
                decT = sb("decT", [128, 34], F32, ps)
                attm = [(('attm', i), sb(f"attmA{i}", [128, 128], BF16, ps)) for i in range(2)]
                S32 = [sb(f"S32_{i}", [128, 128], F32, ps) for i in range(2)]
                Sbf = [[sb(f"Sbf{i}_{k}", [128, 128], BF16, ps) for k in range(2)] for i in range(2)]
                osq = [(('osq', i), sb(f"osqA{i}", [128, 256], BF16, ps)) for i in range(2)]
                ort = ([('ebT', 0, 0), ('ebT', 0, 1)], ebT[:, 0:256])
                ot = ([('ebT', 0, 2), ('ebT', 0, 3)], ebT[:, 256:512])
                for ti, (p0, n) in enumerate(TILES_BLK):
                    pb = B[ti % 2]
                    for k in range(8):
                        cx.op('pe', lambda e: e.matmul(pb[1][0:16, :n], wglr[:, k, :], xn[:, k, p0:p0 + n], start=(k == 0), stop=(k == 7)),
                              reads=['wglr'] + blk_keys('xn', k, p0, n), writes=[pb[0]], inc=(k == 7))
                    cx.op('act', lambda e: e.activation(out=glrT[:, p0:p0 + n], in_=pb[1][0:16, :n], func=AF.Copy),
                          reads=[pb[0]], writes=blk_keys('glrT', 0, p0, n))
                if DBG.get('a_cut', 99) <= 1:
                    return
                for p in range(2):
                    if p > 0:
                        cx.dma('pool', 'w1', wq[:], wv[:, :, O_GQ + 128 * p:O_GQ + 128 * p + 128], writes=['wq'])
                        cx.dma('pool', 'w1', wk[:], wv[:, :, O_GK + 128 * p:O_GK + 128 * p + 128], writes=['wk'])
                        cx.dma('pool', 'w1', wvv[:], wv[:, :, O_GV + 256 * p:O_GV + 256 * p + 256], writes=['wvv'])
                    def g2_pg(b):
                        q0 = 128 * b
                        pg = B[0]
                        cx.op('pe', lambda e: e.matmul(pg[1][:, 0:128], glrT[:, q0:q0 + 128], wg2[:, 128 * p:128 * p + 128], start=True, stop=False),
                              reads=[('glrT', 0, b), 'wg2'], writes=[pg[0]], inc=False)
                        cx.op('pe', lambda e: e.matmul(pg[1][:, 0:128], g.ones_f[0:1, :], bg[0:1, 128 * p:128 * p + 128], start=False, stop=True),
                              reads=['bg'], writes=[pg[0]])
                        spb, e1b = sp[b % 2], e1[b % 2]
                        cx.op('act', lambda e: e.activation(out=e1b[1][:], in_=pg[1][:, 0:128], func=AF.Exp, scale=-1.0),
                              reads=[pg[0]], writes=[e1b[0]])
                        cx.op('act', lambda e: e.activation(out=spb[1][:], in_=e1b[1][:], func=AF.Ln, bias=g.one_ap, scale=1.0),
                              reads=[e1b[0]], writes=[spb[0]])
                        if b == 0:
                            cx.op('pool', lambda e: e.memset(spb[1][0:64, :], 0.0), reads=[spb[0]], writes=[spb[0]])
                            cx.op('pool', lambda e: e.memset(spb[1][64:PAD, :], 0.0), reads=[spb[0]], writes=[spb[0]])

                    g2_pg(0)
                    for b in range(17):
                        q0 = 128 * b
                        pbt, pbT, pk, pvv = B[1], B[2], B[3], B[4 + b % 2]
                        spb, embb = sp[b % 2], emb[b % 2]
                        for k in range(8):
                            cx.op('pe', lambda e: e.matmul(pk[1][:, 0:128], xn[:, k, q0:q0 + 128], wk[:, k, :], start=(k == 0), stop=(k == 7)),
                                  reads=['wk', ('xn', k, b)], writes=[pk[0]], inc=(k == 7))
                        for k in range(8):
                            cx.op('pe', lambda e: e.matmul(pvv[1][:, 0:256], xn[:, k, q0:q0 + 128], wvv[:, k, :], start=(k == 0), stop=(k == 7)),
                                  reads=['wvv', ('xn', k, b)], writes=[pvv[0]], inc=(k == 7))
                        cx.op('act', lambda e: e.activation(out=vtok[:, b, :], in_=pvv[1][:, 0:256], func=AF.Copy),
                              reads=[pvv[0]], writes=[('vtok', b)])
                        if b + 1 < 17:
                            g2_pg(b + 1)
                        cx.op('pe', lambda e: e.matmul(pbt[1][:, 0:128], mc2[:], spb[1][:], start=True, stop=True),
                              reads=['mc2', spb[0]], writes=[pbt[0]])
                        cx.op('act', lambda e: e.activation(out=embb[1][:], in_=pbt[1][:, 0:128], func=AF.Exp, scale=1.0),
                              reads=[pbt[0]], writes=[embb[0]])
                        cx.op('pe', lambda e: e.matmul(pbT[1][:, 0:128], spb[1][:], mc[:], start=True, stop=True),
                              reads=['mc', spb[0]], writes=[pbT[0]])
                        cx.op('act', lambda e: e.activation(out=ebT[:, q0:q0 + 128], in_=pbT[1][:, 0:128], func=AF.Copy),
                              reads=[pbT[0]], writes=[('ebT', 0, b)])
                        for hf in range(2):
                            cx.op('act', lambda e: e.activation(out=decT[:, 2 * b + hf:2 * b + hf + 1], in_=pbT[1][:, 64 * hf + 63:64 * hf + 64], func=AF.Exp),
                                  reads=[pbT[0]], writes=['decT'], inc=(hf == 1))
                        cx.op('dve', lambda e: e.tensor_tensor(out=kttok[:, b, :], in0=pk[1][:, 0:128], in1=embb[1][:], op=ALU.mult),
                              reads=[pk[0], embb[0]], writes=[('kttok', b)])
                    if DBG.get('a_cut', 99) <= 2:
                        continue
                    cx.dma('pool', 'w1', wr[:], wv[:, :, O_GR + 256 * p:O_GR + 256 * p + 256], writes=['wvv'])
                    for ti, (p0, n) in enumerate(TILES_BLK):
                        pq, pkk = B[6], B[7]
                        rc, rc2 = rec[0], rec[1]
                        for k in range(8):
                            cx.op('pe', lambda e: e.matmul(pq[1][:, :n], wq[:, k, :], xn[:, k, p0:p0 + n], start=(k == 0), stop=(k == 7)),
                                  reads=['wq'] + blk_keys('xn', k, p0, n), writes=[pq[0]], inc=(k == 7))
                        cx.op('act', lambda e: e.activation(out=rc[1][:, :n], in_=ebT[:, p0:p0 + n], func=AF.Exp),
                              reads=blk_keys('ebT', 0, p0, n), writes=[rc[0]])
                        cx.op('dve', lambda e: e.scalar_tensor_tensor(out=qtT[:, p0:p0 + n], in0=pq[1][:, :n], scalar=0.125, in1=rc[1][:, :n],
                                                                       op0=ALU.mult, op1=ALU.mult),
                              reads=[pq[0], rc[0]], writes=blk_keys('qtT', 0, p0, n))
                        for k in range(8):
                            cx.op('pe', lambda e: e.matmul(pkk[1][:, :n], wk[:, k, :], xn[:, k, p0:p0 + n], start=(k == 0), stop=(k == 7)),
                                  reads=['wk'] + blk_keys('xn', k, p0, n), writes=[pkk[0]], inc=(k == 7))
                        cx.op('act', lambda e: e.activation(out=rc2[1][:, :n], in_=ebT[:, p0:p0 + n], func=AF.Exp, scale=-1.0),
                              reads=blk_keys('ebT', 0, p0, n), writes=[rc2[0]])
                        cx.op('dve', lambda e: e.tensor_tensor(out=ktT[:, p0:p0 + n], in0=pkk[1][:, :n], in1=rc2[1][:, :n], op=ALU.mult),
                              reads=[pkk[0], rc2[0]], writes=blk_keys('ktT', 0, p0, n))
                        for j in range(2):
                            pr = B[4 + j]
                            for k in range(8):
                                cx.op('pe', lambda e: e.matmul(pr[1][:, :n], wr[:, k, 128 * j:128 * j + 128], xn[:, k, p0:p0 + n], start=(k == 0), stop=(k == 7)),
                                      reads=['wvv'] + blk_keys('xn', k, p0, n), writes=[pr[0]], inc=(k == 7))
                            cx.op('act', lambda e: e.activation(out=srT[:, j, p0:p0 + n], in_=pr[1][:, :n], func=AF.Silu),
                                  reads=[pr[0]], writes=blk_keys('srT', j, p0, n))
                    if DBG.get('a_cut', 99) <= 3:
                        continue
                    cx.op('dve', lambda e: e.memset(S32[0][:], 0.0), writes=[('S32', 0)])
                    cx.op('pool', lambda e: e.memset(Sbf[0][0][:], 0.0), writes=[('Sbf', 0, 0)])

                    def Oap(b):
                        sl = b % 4
                        return B[sl % 2][0], B[sl % 2][1][:, 256 * (sl // 2):256 * (sl // 2) + 256]

                    def g_att(b):
                        q0 = 128 * b
                        for j in range(2):
                            pat = B[2 + j]
                            am = attm[j]
                            cx.op('pe', lambda e: e.matmul(pat[1][:, 0:128], ktT[64 * j:64 * j + 64, q0:q0 + 128], qtT[64 * j:64 * j + 64, q0:q0 + 128], start=True, stop=True),
                                  reads=[('ktT', 0, b), ('qtT', 0, b)], writes=[pat[0]])
                            cx.op('dve', lambda e: e.tensor_tensor(out=am[1][:], in0=pat[1][:, 0:128], in1=gmask[:], op=ALU.mult),
                                  reads=[pat[0], 'gmask'], writes=[am[0]])

                    def g_kv(b, hf):
                        pkv = B[4 + hf]
                        cx.op('pe', lambda e: e.matmul(pkv[1][:, 0:256], kttok[64 * hf:64 * hf + 64, b, :], vtok[64 * hf:64 * hf + 64, b, :], start=True, stop=True),
                              reads=[('kttok', b), ('vtok', b)], writes=[pkv[0]])

                    def g_upd(b, hf):
                        pkv = B[4 + hf]
                        ci = 2 * b + hf
                        dec = decT[:, ci:ci + 1]
                        src, dst = S32[ci % 2], S32[(ci + 1) % 2]
                        sk, dk = ('S32', ci % 2), ('S32', (ci + 1) % 2)
                        for j in range(2):
                            cx.op('dve', lambda e: e.scalar_tensor_tensor(out=dst[64 * j:64 * j + 64, :], in0=src[64 * j:64 * j + 64, :],
                                                                           scalar=decT[64 * j:64 * j + 64, ci:ci + 1], in1=pkv[1][64 * j:64 * j + 64, 128 * j:128 * j + 128],
                                                                           op0=ALU.mult, op1=ALU.add),
                                  reads=[pkv[0], sk, 'decT'], writes=[dk], inc=(j == 1))
                        sbk = (1, b % 2) if hf == 0 else (0, (b + 1) % 2)
                        cx.op('act', lambda e: e.activation(out=Sbf[sbk[0]][sbk[1]][:], in_=dst[:], func=AF.Copy), reads=[dk], writes=[('Sbf',) + sbk])

                    def g_omm(b):
                        q0 = 128 * b
                        ok, oap = Oap(b)
                        for j in range(2):
                            am = attm[j]
                            cx.op('pe', lambda e: e.matmul(oap[:, 128 * j:128 * j + 128], vtok[:, b, 128 * j:128 * j + 128], am[1][:], start=True, stop=False),
                                  reads=[('vtok', b), am[0]], writes=[ok], inc=False)
                            for hf in range(2):
                                c0 = q0 + 64 * hf
                                cx.op('pe', lambda e: e.matmul(oap[:, 128 * j + 64 * hf:128 * j + 64 * hf + 64], Sbf[hf][b % 2][64 * j:64 * j + 64, :], qtT[64 * j:64 * j + 64, c0:c0 + 64],
                                                               start=False, stop=(hf == 1)),
                                      reads=[('Sbf', hf, b % 2), ('qtT', 0, b)], writes=[ok], inc=(hf == 1))

                    def g_sq(b):
                        ok, oap = Oap(b)
                        os_ = osq[b % 2]
                        cx.op('act', lambda e: e.activation(out=os_[1][:], in_=oap, func=AF.Square), reads=[ok], writes=[os_[0]])

                    def g_ones(b):
                        pss = B[6 + b % 2]
                        os_ = osq[b % 2]
                        cx.op('pe', lambda e: e.matmul(pss[1][:, 0:256], g.ones_bf[:], os_[1][:], start=True, stop=True), reads=[os_[0]], writes=[pss[0]])

                    def g_fin(b):
                        q0 = 128 * b
                        ok, oap = Oap(b)
                        pss = B[6 + b % 2]
                        cx.op('act', lambda e: e.activation(out=ort[1], in_=pss[1][:, 0:256], func=AF.Ln, bias=g.eps_ap, scale=1.0 / 128),
                              reads=[pss[0]], writes=ort[0])
                        cx.op('act', lambda e: e.activation(out=ort[1], in_=ort[1], func=AF.Exp, scale=-0.5), reads=ort[0], writes=ort[0])
                        cx.op('dve', lambda e: e.scalar_tensor_tensor(out=ot[1], in0=oap, scalar=g.vecs[:, V_ON:V_ON + 1], in1=ort[1],
                                                                       op0=ALU.mult, op1=ALU.mult),
                              reads=[ok] + ort[0], writes=ot[0])
                        cx.op('pool', lambda e: e.tensor_tensor(out=mix[:, 2 * p:2 * p + 2, q0:q0 + 128], in0=ot[1].rearrange("p (j c) -> p j c", j=2),
                                                                in1=srT[:, :, q0:q0 + 128], op=ALU.mult),
                              reads=ot[0] + [('srT', 0, b), ('srT', 1, b)], writes=[('mix', 2 * p, b), ('mix', 2 * p + 1, b)])

                    g_att(0)
                    g_kv(0, 0)
                    g_kv(0, 1)
                    for it in range(17 + 3):
                        b = it
                        if b < 17:
                            g_upd(b, 0)
                            if b + 1 < 17:
                                g_kv(b + 1, 0)
                            g_omm(b)
                            g_upd(b, 1)
                            if b + 1 < 17:
                                g_kv(b + 1, 1)
                                g_att(b + 1)
                        if 0 <= it - 1 < 17:
                            g_sq(it - 1)
                        if 0 <= it - 2 < 17:
                            g_ones(it - 2)
                        if 0 <= it - 3 < 17:
                            g_fin(it - 3)
                cx.barrier()
            if DBG.get('a_cut', 99) <= 4:
                return
        with ExitStack() as ps:
            rot = sb("rotA", [128, 128], F32, ps)
            bd = sb("bdA", [128, 128], BF16, ps)
            smask = sb("smaskA", [128, 4, 128], F32, ps)
            cosT = sb("cosA", [128, NPOS], F32, ps)
            sinT = sb("sinA", [128, NPOS], F32, ps)
            sinkexp = sb("sinkexpA", [128, 8], F32, ps)
            qr = sb("qrA", [128, 2, NPOS], BF16, ps)
            kr = sb("krA", [128, NPOS], BF16, ps)
            svt = sb("svtA", [128, 17, 128], BF16, ps)
            wsq = sb("wsqA", [128, 8, 256], BF16, ps)
            wsk = sb("wskA", [128, 8, 128], BF16, ps)
            wsv = sb("wsvA", [128, 8, 128], BF16, ps)
            zsq = [(('zsq', i), sb(f"zsqA{i}", [128, 512], BF16, ps)) for i in range(2)]
            zrt = [(('zrt', i), sb(f"zrtA{i}", [128, 512], F32, ps)) for i in range(2)]
            qn = [(('qn', i), sb(f"qnA{i}", [128, 512], F32, ps)) for i in range(2)]
            t1 = [(('t1', i), sb(f"t1A{i}", [128, 512], F32, ps)) for i in range(1)]
            t2 = [(('t2', i), sb(f"t2A{i}", [128, 512], F32, ps)) for i in range(1)]
            identbf = sb("identbfA", [128, 128], BF16, ps)
            mb = sb("mbA", [128, 4, 256], BF16, ps)
            em = [(('em', i), sb(f"emA{i}", [128, 512], BF16, ps)) for i in range(2)]
            dns = [(('dns', i), sb(f"dnsA{i}", [128, 512], F32, ps)) for i in range(2)]
            cx.dma('sp', 'io', rot[:], consts[:, C_ROT:C_ROT + 128], writes=['rot'])
            cx.dma('sp', 'io', smask[:], consts[:, C_SMASK:C_SMASK + 2048].rearrange("p (t c) -> p t c", t=4)[:, :, 0:128], writes=['smask'])
            cx.dma('sp', 'io', cosT[:], consts[:, C_COS:C_COS + NPOS], writes=['cos'])
            cx.dma('sp', 'io', sinT[:], consts[:, C_SIN:C_SIN + NPOS], writes=['sin'])
            cx.op('pool', lambda e: e.memset(bd[:], 0.0), writes=['bd'])
            cx.op('pool', lambda e: e.memset(bd[0:64, 0:64], 1.0), reads=['bd'], writes=['bd'])
            cx.op('pool', lambda e: e.memset(bd[64:128, 64:128], 1.0), reads=['bd'], writes=['bd'])
            cx.op('act', lambda e: e.activation(out=sinkexp[:], in_=g.vecs[:, V_SINK:V_SINK + 8], func=AF.Exp), writes=['sinkexp'])
            cx.op('dve', lambda e: e.tensor_copy(out=identbf[:], in_=g.ident[:]), writes=['identbf'])
            for r in range(2):
                cx.op('dve', lambda e: e.tensor_scalar(out=mb[:, :, 128 * r:128 * r + 128], in0=smask[:], scalar1=-1.0, scalar2=30000.0, op0=ALU.add, op1=ALU.mult),
                      reads=['smask'], writes=['mb'])
            for hk in range(2):
                cx.dma('pool', 'w0', wsq[:], wv[:, :, O_SQ + 256 * hk:O_SQ + 256 * hk + 256], writes=['wsq'])
                for r in range(2):
                    cx.dma('pool', 'w0', wsk[:, :, 64 * r:64 * r + 64], wv[:, :, O_SK + 64 * hk:O_SK + 64 * hk + 64], writes=['wsk'])
                    cx.dma('pool', 'w0', wsv[:, :, 64 * r:64 * r + 64], wv[:, :, O_SV + 64 * hk:O_SV + 64 * hk + 64], writes=['wsv'])
                its = [(ci, p0, n) for ci in range(3) for (p0, n) in TILES_BLK]
                PZ, PSS, PROT = [B[0], B[1], B[2]], [B[3], B[4]], B[5]

                def q_z(i):
                    ci, p0, n = its[i]
                    pz = PZ[i % 3]
                    for k in range(8):
                        lhs = wsq[:, k, 128 * ci:128 * ci + 128] if ci < 2 else wsk[:, k, :]
                        cx.op('pe', lambda e: e.matmul(pz[1][:, :n], lhs, xn[:, k, p0:p0 + n], start=(k == 0), stop=(k == 7)),
                              reads=['wsq', 'wsk'] + blk_keys('xn', k, p0, n), writes=[pz[0]], inc=(k == 7))

                def q_mid(i):
                    ci, p0, n = its[i]
                    pz, pss = PZ[i % 3], PSS[i % 2]
                    zs, zr, qq = zsq[i % 2], zrt[i % 2], qn[i % 2]
                    cx.op('act', lambda e: e.activation(out=zs[1][:, :n], in_=pz[1][:, :n], func=AF.Square), reads=[pz[0]], writes=[zs[0]])
                    cx.op('pe', lambda e: e.matmul(pss[1][:, :n], bd[:], zs[1][:, :n], start=True, stop=True), reads=['bd', zs[0]], writes=[pss[0]])
                    cx.op('act', lambda e: e.activation(out=zr[1][:, :n], in_=pss[1][:, :n], func=AF.Ln, bias=g.eps_ap, scale=1.0 / 64),
                          reads=[pss[0]], writes=[zr[0]])
                    cx.op('act', lambda e: e.activation(out=zr[1][:, :n], in_=zr[1][:, :n], func=AF.Exp, scale=-0.5), reads=[zr[0]], writes=[zr[0]])
                    gcol = V_QN if ci < 2 else V_KN
                    cx.op('dve', lambda e: e.scalar_tensor_tensor(out=qq[1][:, :n], in0=pz[1][:, :n], scalar=g.vecs[:, gcol:gcol + 1], in1=zr[1][:, :n],
                                                                   op0=ALU.mult, op1=ALU.mult),
                          reads=[pz[0], zr[0]], writes=[qq[0]])

                def q_tail(i):
                    ci, p0, n = its[i]
                    qq, a1, a2 = qn[i % 2], t1[0], t2[0]
                    cx.op('pe', lambda e: e.matmul(PROT[1][:, :n], rot[:], qq[1][:, :n], start=True, stop=True), reads=['rot', qq[0]], writes=[PROT[0]])
                    cx.op('dve', lambda e: e.tensor_tensor(out=a1[1][:, :n], in0=qq[1][:, :n], in1=cosT[:, p0:p0 + n], op=ALU.mult),
                          reads=[qq[0], 'cos'], writes=[a1[0]])
                    cx.op('dve', lambda e: e.tensor_tensor(out=a2[1][:, :n], in0=PROT[1][:, :n], in1=sinT[:, p0:p0 + n], op=ALU.mult),
                          reads=[PROT[0], 'sin'], writes=[a2[0]])
                    dst = qr[:, ci, p0:p0 + n] if ci < 2 else kr[:, p0:p0 + n]
                    dkeys = blk_keys('qr', ci, p0, n) if ci < 2 else blk_keys('kr', 0, p0, n)
                    cx.op('pool', lambda e: e.tensor_tensor(out=dst, in0=a1[1][:, :n], in1=a2[1][:, :n], op=ALU.add),
                          reads=[a1[0], a2[0]], writes=dkeys)

                def q_v(b):
                    pv = B[6 + b % 2]
                    for k in range(8):
                        cx.op('pe', lambda e: e.matmul(pv[1][:, 0:128], xn[:, k, 128 * b:128 * b + 128], wsv[:, k, :], start=(k == 0), stop=(k == 7)),
                              reads=['wsv', ('xn', k, b)], writes=[pv[0]], inc=(k == 7))
                    cx.op('act', lambda e: e.activation(out=svt[:, b, :], in_=pv[1][:, 0:128], func=AF.Copy), reads=[pv[0]], writes=[('svt', b)])

                q_z(0)
                for i in range(len(its)):
                    if i + 1 < len(its):
                        q_z(i + 1)
                    q_mid(i)
                    if i >= 1:
                        q_tail(i - 1)
                    q_v(i)
                q_tail(len(its) - 1)
                q_v(15)
                q_v(16)
                if DBG.get('a_cut', 99) <= 6:
                    continue
                units = []
                for qb in range(17):
                    kbs = [qb - 1, qb] if qb >= 1 else [0]
                    for idx, kb in enumerate(kbs):
                        units.append((qb, kb, idx, len(kbs)))

                def s_scores(n):
                    qb, kb, idx, nk = units[n]
                    q0, k0 = 128 * qb, 128 * kb
                    scA, scB = B[(2 * n) % 4], B[(2 * n + 1) % 4]
                    mt = (0 if kb == qb else 1) + (2 if kb == 0 else 0)
                    for c, j in enumerate(JORD):
                        hp = 64 * (j % 2)
                        sc = scA if j % 2 == 0 else scB
                        cc = c % 2
                        cx.op('pe', lambda e: e.matmul(sc[1][:, 128 * cc:128 * cc + 128], kr[hp:hp + 64, k0:k0 + 128], qr[hp:hp + 64, j // 2, q0:q0 + 128],
                                                       start=(cc == 0), stop=False),
                              reads=[('kr', 0, kb), ('qr', j // 2, qb)], writes=[sc[0]], inc=False)
                    for sc in (scA, scB):
                        cx.op('pe', lambda e: e.matmul(sc[1][:, 0:256], identbf[:], mb[:, mt, :], start=False, stop=True),
                              reads=['identbf', 'mb'], writes=[sc[0]])

                def s_pv(n):
                    qb, kb, idx, nk = units[n]
                    scA, scB = B[(2 * n) % 4], B[(2 * n + 1) % 4]
                    Oo, Dn = B[4 + qb % 2], B[6 + qb % 2]
                    emm = em[n % 2]
                    cx.op('act', lambda e: e.activation(out=emm[1][:, 0:256], in_=scA[1][:, 0:256], func=AF.Exp, scale=0.125), reads=[scA[0]], writes=[emm[0]])
                    cx.op('act', lambda e: e.activation(out=emm[1][:, 256:512], in_=scB[1][:, 0:256], func=AF.Exp, scale=0.125), reads=[scB[0], emm[0]], writes=[emm[0]])
                    cx.op('pe', lambda e: e.matmul(Oo[1][:], svt[:, kb, :], emm[1][:], start=(idx == 0), stop=(idx == nk - 1)),
                          reads=[('svt', kb), emm[0]], writes=[Oo[0]])
                    cx.op('pe', lambda e: e.matmul(Dn[1][:], g.ones_bf[:], emm[1][:], start=(idx == 0), stop=(idx == nk - 1)),
                          reads=[emm[0]], writes=[Dn[0]])

                def s_fin1(qb):
                    Dn = B[6 + qb % 2]
                    dn = dns[qb % 2]
                    for c, j in enumerate(JORD):
                        cx.op('dve', lambda e: e.tensor_scalar(out=dn[1][:, 128 * c:128 * c + 128], in0=Dn[1][:, 128 * c:128 * c + 128],
                                                                scalar1=sinkexp[:, 4 * hk + j:4 * hk + j + 1], scalar2=None, op0=ALU.add),
                              reads=[Dn[0], 'sinkexp'], writes=[dn[0]], inc=(c == 3))
                    cx.op('act', lambda e: e.activation(out=dn[1][:], in_=dn[1][:], func=AF.Ln), reads=[dn[0]], writes=[dn[0]])
                    cx.op('act', lambda e: e.activation(out=dn[1][:], in_=dn[1][:], func=AF.Exp, scale=-1.0), reads=[dn[0]], writes=[dn[0]])

                def s_fin2(qb):
                    q0 = 128 * qb
                    Oo = B[4 + qb % 2]
                    dn = dns[qb % 2]
                    for c, j in enumerate(JORD):
                        hp = 64 * (j % 2)
                        cx.op('dve', lambda e: e.tensor_tensor(out=mix[hp:hp + 64, 4 + 2 * hk + j // 2, q0:q0 + 128],
                                                                in0=Oo[1][hp:hp + 64, 128 * c:128 * c + 128], in1=dn[1][hp:hp + 64, 128 * c:128 * c + 128], op=ALU.mult),
                              reads=[Oo[0], dn[0]], writes=[('mix', 4 + 2 * hk + j // 2, qb)], inc=(c == 3))

                s_scores(0)
                for n, (qb, kb, idx, nk) in enumerate(units):
                    if idx == 0 and qb >= 2:
                        s_fin2(qb - 2)
                    if n + 1 < len(units):
                        s_scores(n + 1)
                    s_pv(n)
                    if idx == nk - 1 and qb >= 1:
                        s_fin1(qb - 1)
                s_fin2(15)
                s_fin1(16)
                s_fin2(16)
            cx.barrier()
        if DBG.get('a_cut', 99) <= 7:
            return
        with ExitStack() as ps:
            wo = sb("woA", [128, 8, D], BF16, ps)
            cx.dma('pool', 'w1', wo[:], w_out.rearrange("(k p) d -> p k d", p=128), writes=['wo'])
            it = 0
            for (p0, n) in TILES_ALL:
                for d in range(8):
                    pb = B[it % 4]
                    it += 1
                    for c in range(8):
                        cx.op('pe', lambda e: e.matmul(pb[1][:, :n], wo[:, c, d * 128:(d + 1) * 128], mix[:, c, p0:p0 + n], start=(c == 0), stop=(c == 7)),
                              reads=['wo'] + blk_keys('mix', c, p0, n), writes=[pb[0]], inc=(c == 7))
                    cx.op('dve', lambda e: e.tensor_tensor(out=g.h[:, d, p0:p0 + n], in0=pb[1][:, :n], in1=g.h[:, d, p0:p0 + n], op=ALU.add),
                          reads=[pb[0]] + blk_keys('h', d, p0, n), writes=blk_keys('h', d, p0, n))
            cx.barrier()


def emit_phase_c(cx, g, sb, w_pw1, w_pw2):
    nc = cx.nc
    w1v = w_pw1.rearrange("(k p) f -> p k f", p=128)
    with ExitStack() as pc:
        u = sb("uC", [128, 8, NPOS], BF16, pc)
        w2s = sb("w2C", [128, 8, D], BF16, pc)
        cx.op('pool', lambda e: e.memset(u[:, :, 0:128], 0.0), writes=[('u', c, 0) for c in range(8)])
        with ExitStack() as ps:
            xn = sb("xnC", [128, 8, NPOS], BF16, ps)
            sq = sb("sqC", [128, 8, 512], BF16, ps)
            rstd = [(('rstd', i), sb(f"rstdC{i}", [128, 512], F32, ps)) for i in range(2)]
            wa = [sb(f"waC{i}", [128, 8, 128], BF16, ps) for i in range(2)]
            wg = [sb(f"wgC{i}", [128, 8, 128], BF16, ps) for i in range(2)]
            sg = [(('sg', i), sb(f"sgC{i}", [128, 512], F32, ps)) for i in range(2)]
            def norm_c(ti):
                emit_rmsnorm(cx, g, V_CNORM, [TILES_ALL[ti]], xn, 'xn', sq, [rstd[ti % 2]], [g.psx[ti % 2]])
            n_it = 0
            for cc in range(8):
                s = cc % 2
                cx.dma('pool', f'w{s}', wa[s][:], w1v[:, :, cc * 128:(cc + 1) * 128], writes=[('wa', s)])
                cx.dma('pool', f'w{s}', wg[s][:], w1v[:, :, D + cc * 128:D + (cc + 1) * 128], writes=[('wg', s)])
                if cc == 0:
                    norm_c(0)
                if cc == 1:
                    cx.dma('pool', 'w1', w2s[:], w_pw2.rearrange("(k p) d -> p k d", p=128), writes=['w2C'])
                for ti_c, (p0, n) in enumerate(TILES_ALL):
                    if cc == 0 and ti_c + 1 < len(TILES_ALL):
                        norm_c(ti_c + 1)
                    pa = g.ps1[n_it % 2]
                    pg = g.ps3[n_it % 2]
                    sgb = sg[n_it % 2]
                    n_it += 1
                    for k in range(8):
                        cx.op('pe', lambda e: e.matmul(pa[1][:, :n], wa[s][:, k, :], xn[:, k, p0:p0 + n], start=(k == 0), stop=(k == 7)),
                              reads=[('wa', s)] + blk_keys('xn', k, p0, n), writes=[pa[0]], inc=(k == 7))
                    for k in range(8):
                        cx.op('pe', lambda e: e.matmul(pg[1][:, :n], wg[s][:, k, :], xn[:, k, p0:p0 + n], start=(k == 0), stop=(k == 7)),
                              reads=[('wg', s)] + blk_keys('xn', k, p0, n), writes=[pg[0]], inc=(k == 7))
                    cx.op('act', lambda e: e.activation(out=sgb[1][:, :n], in_=pg[1][:, :n], func=AF.Sigmoid),
                          reads=[pg[0]], writes=[sgb[0]])
                    if p0 == PAD:
                        cx.op('dve', lambda e: e.tensor_tensor(out=u[:, cc, p0:p0 + n], in0=pa[1][:, :n], in1=sgb[1][:, :n], op=ALU.mult),
                              reads=[pa[0], sgb[0], ('u', cc, 0)], writes=[('u', cc, 0)])
                    else:
                        cx.op('dve', lambda e: e.tensor_tensor(out=u[:, cc, p0:p0 + n], in0=pa[1][:, :n], in1=sgb[1][:, :n], op=ALU.mult),
                              reads=[pa[0], sgb[0]], writes=blk_keys('u', cc, p0, n))
            cx.barrier()
        with ExitStack() as ps:
            if DBG.get('c_cut') == 1:
                return
            dg = [sb(f"dgC{i}", [128, 31, 128], BF16, ps) for i in range(2)]
            y32s = [sb(f"y32C{i}", [128, 8, 512], F32, ps) for i in range(2)]
            ybf = sb("ybfC", [128, 8, 512], BF16, ps)
            ysq = sb("ysqC", [128, 8, 512], BF16, ps)
            mean = sb("meanC", [128, 512], F32, ps)
            msq = sb("msqC", [128, 512], F32, ps)
            rstd = sb("rstdC2", [128, 512], F32, ps)
            tt = [(('tt', i), sb(f"ttC{i}", [128, 512], F32, ps)) for i in range(2)]
            v = sb("vC", [128, 8, 512], BF16, ps)
            def c_conv_mm(ti, cc):
                p0, n = TILES_REAL[ti]
                s = (ti * 8 + cc) % 2
                py = g.ps1[(ti * 8 + cc) % 2]
                col = V_WDW + cc * 31
                cx.op('dve', lambda e: e.tensor_tensor(out=dg[s][:], in0=g.ident[:].unsqueeze(1).to_broadcast([128, 31, 128]),
                                                        in1=g.vecs[:, col:col + 31].unsqueeze(2).to_broadcast([128, 31, 128]), op=ALU.mult),
                      reads=[], writes=[('dg', s)])
                for j in range(31):
                    q0 = p0 - 30 + j
                    cx.op('pe', lambda e: e.matmul(py[1][:, :n], dg[s][:, j, :], u[:, cc, q0:q0 + n], start=(j == 0), stop=(j == 30)),
                          reads=[('dg', s)] + blk_keys('u', cc, p0 - 30, n + 30), writes=[py[0]], inc=(j == 30))

            def c_conv_ev(ti, cc):
                p0, n = TILES_REAL[ti]
                y32 = y32s[ti % 2]
                py = g.ps1[(ti * 8 + cc) % 2]
                bcol = V_CBDW + cc
                cx.op('act', lambda e: e.activation(out=y32[:, cc, :], in_=py[1][:, :n], func=AF.Identity, bias=g.vecs[:, bcol:bcol + 1], scale=1.0),
                      reads=[py[0]], writes=[('y32', ti % 2, cc)])
                cx.op('pool', lambda e: e.tensor_copy(out=ybf[:, cc, :], in_=y32[:, cc, :]), reads=[('y32', ti % 2, cc)], writes=[('ybf', cc)])
                cx.op('act', lambda e: e.activation(out=ysq[:, cc, :], in_=y32[:, cc, :], func=AF.Square), reads=[('y32', ti % 2, cc)], writes=[('ysq', cc)])

            def c_stats(ti):
                p0, n = TILES_REAL[ti]
                psm = g.psx[0]
                psq = g.psx[1]
                for cc in range(8):
                    cx.op('pe', lambda e: e.matmul(psm[1][:, :n], g.ones_bf[:], ybf[:, cc, :], start=(cc == 0), stop=(cc == 7)),
                          reads=[('ybf', cc)], writes=[psm[0]], inc=(cc == 7))
                for cc in range(8):
                    cx.op('pe', lambda e: e.matmul(psq[1][:, :n], g.ones_bf[:], ysq[:, cc, :], start=(cc == 0), stop=(cc == 7)),
                          reads=[('ysq', cc)], writes=[psq[0]], inc=(cc == 7))
                cx.op('act', lambda e: e.activation(out=mean[:], in_=psm[1][:], func=AF.Copy, scale=1.0 / D), reads=[psm[0]], writes=['mean'])
                cx.op('act', lambda e: e.activation(out=msq[:], in_=psm[1][:], func=AF.Square, scale=1.0 / D), reads=[psm[0]], writes=['msq'])
                cx.op('dve', lambda e: e.scalar_tensor_tensor(out=rstd[:], in0=psq[1][:], scalar=1.0 / D, in1=msq[:], op0=ALU.mult, op1=ALU.subtract),
                      reads=[psq[0], 'msq'], writes=['rstdC'])
                cx.op('act', lambda e: e.activation(out=rstd[:], in_=rstd[:], func=AF.Ln, bias=g.eps_ap, scale=1.0), reads=['rstdC'], writes=['rstdC'])
                cx.op('act', lambda e: e.activation(out=rstd[:], in_=rstd[:], func=AF.Exp, scale=-0.5), reads=['rstdC'], writes=['rstdC'])

            def c_norm(ti, cc):
                y32 = y32s[ti % 2]
                t = tt[cc % 2]
                cx.op('dve', lambda e: e.tensor_tensor(out=t[1][:], in0=y32[:, cc, :], in1=mean[:], op=ALU.subtract),
                      reads=[('y32', ti % 2, cc), 'mean'], writes=[t[0]])
                cx.op('pool', lambda e: e.tensor_tensor(out=t[1][:], in0=t[1][:], in1=rstd[:], op=ALU.mult),
                      reads=[t[0], 'rstdC'], writes=[t[0]])
                gc, bc = V_CLNG + cc, V_CLNB + cc
                cx.op('dve', lambda e: e.tensor_scalar(out=t[1][:], in0=t[1][:], scalar1=g.vecs[:, gc:gc + 1], scalar2=g.vecs[:, bc:bc + 1],
                                                        op0=ALU.mult, op1=ALU.add),
                      reads=[t[0]], writes=[t[0]])
                cx.op('act', lambda e: e.activation(out=v[:, cc, :], in_=t[1][:], func=AF.Silu),
                      reads=[t[0]], writes=[('v', cc)])

            def c_pw2(ti):
                p0, n = TILES_REAL[ti]
                for d in range(8):
                    pb = g.pso[d % 2]
                    for cc in range(8):
                        cx.op('pe', lambda e: e.matmul(pb[1][:, :n], w2s[:, cc, d * 128:(d + 1) * 128], v[:, cc, :], start=(cc == 0), stop=(cc == 7)),
                              reads=['w2C', ('v', cc)], writes=[pb[0]], inc=(cc == 7))
                    cx.op('dve', lambda e: e.tensor_tensor(out=g.h[:, d, p0:p0 + n], in0=pb[1][:, :n], in1=g.h[:, d, p0:p0 + n], op=ALU.add),
                          reads=[pb[0]] + blk_keys('h', d, p0, n), writes=blk_keys('h', d, p0, n))

            for cc in range(8):
                c_conv_mm(0, cc)
                c_conv_ev(0, cc)
            c_stats(0)
            for ti in range(4):
                for cc in range(8):
                    if ti + 1 < 4:
                        c_conv_mm(ti + 1, cc)
                    c_norm(ti, cc)
                    if ti + 1 < 4:
                        c_conv_ev(ti + 1, cc)
                c_pw2(ti)
                if ti + 1 < 4:
                    c_stats(ti + 1)
            cx.barrier()


def emit_phase_d(cx, g, sb, w_router, m_w1, m_w3, m_w2):
    nc = cx.nc
    with ExitStack() as po:
        xn = sb("xnD", [128, 8, NPOS], BF16, po)
        gates = sb("gatesD", [128, 16, NE], F32, po)
        sq = sb("sqD", [128, 4, 512], BF16, po)
        rstd = [(('rstd', i), sb(f"rstdD{i}", [128, 512], F32, po)) for i in range(1)]
        x32 = (('x32', 0), sb("x32D0", [128, 8, 512], F32, po))
        wr = sb("wrD", [128, 8, NE], F32, po)
        lg = sb("lgD", [128, 4 * NE], F32, po)
        mx8 = sb("mx8D", [128, 4, 8], F32, po)
        ex = sb("exD", [128, 4 * NE], F32, po)
        msk = sb("mskD", [128, 4 * NE], F32, po)
        sm = sb("smD", [128, 4, 4], F32, po)
        dgt = [(('dgt', i), sb(f"dgtD{i}", [128, 128], F32, po)) for i in range(2)]
        Gt = [(('G', i), sb(f"GD{i}", [128, SEQ], F32, po)) for i in range(2)]
        g.w1g = [sb(f"w1gD{i}", [128, 8, 512], BF16, po) for i in range(2)]
        g.w3g = [sb(f"w3gD{i}", [128, 8, 512], BF16, po) for i in range(2)]
        g.w2g = [sb(f"w2gD{i}", [128, 4, 1024], BF16, po) for i in range(2)]
        g.hh = [sb(f"hhD{i}", [128, 4, 512], BF16, po) for i in range(2)]
        g.silu = [(('silu', i), sb(f"siluD{i}", [128, 512], F32, po)) for i in range(3)]

        groups = []
        for e in range(NE):
            groups += ffn_groups(m_w1[e], m_w3[e], m_w2[e], gate=Gt[e % 2])
        slot0 = ffn_load(cx, g, groups[0])
        cx.dma('sp', 'io', wr[:], w_router.rearrange("(k p) e -> p k e", p=128), writes=['wr'])

        def router(ti):
            xb = x32
            pl = g.psx[1]
            for b in range(4):
                for k in range(8):
                    cx.op('pe', lambda e: e.matmul(pl[1][:, b * NE:(b + 1) * NE], xb[1][:, k, b * 128:(b + 1) * 128], wr[:, k, :], start=(k == 0), stop=(k == 7)),
                          reads=[(xb[0], k), 'wr'], writes=[pl[0]], inc=(k == 7))
            lg3 = lg[:].rearrange("p (b e) -> p b e", b=4)
            ex3 = ex[:].rearrange("p (b e) -> p b e", b=4)
            msk3 = msk[:].rearrange("p (b e) -> p b e", b=4)
            cx.op('dve', lambda e: e.tensor_copy(out=lg[:], in_=pl[1][:, :4 * NE]), reads=[pl[0]], writes=['lg'])
            for b in range(4):
                cx.op('dve', lambda e: e.max(out=mx8[:, b, :], in_=lg3[:, b, :]), reads=['lg'], writes=['mx8'], inc=(b == 3))
            m1 = mx8[:, :, 0:1]
            m2 = mx8[:, :, 1:2]
            cx.op('dve', lambda e: e.tensor_tensor(out=ex3, in0=lg3, in1=m1.to_broadcast([128, 4, NE]), op=ALU.subtract),
                  reads=['lg', 'mx8'], writes=['ex'])
            cx.op('act', lambda e: e.activation(out=ex[:], in_=ex[:], func=AF.Exp), reads=['ex'], writes=['ex'])
            cx.op('dve', lambda e: e.tensor_tensor(out=sm[:, :, 0:1], in0=m2, in1=m1, op=ALU.subtract), reads=['mx8'], writes=['sm0'])
            cx.op('act', lambda e: e.activation(out=sm[:, :, 1:2], in_=sm[:, :, 0:1], func=AF.Exp), reads=['sm0'], writes=['sm1'])
            cx.op('dve', lambda e: e.tensor_scalar(out=sm[:, :, 2:3], in0=sm[:, :, 1:2], scalar1=1.0, scalar2=None, op0=ALU.add),
                  reads=['sm1'], writes=['sm2'])
            cx.op('dve', lambda e: e.reciprocal(out=sm[:, :, 3:4], in_=sm[:, :, 2:3]), reads=['sm2'], writes=['sm3'])
            cx.op('dve', lambda e: e.tensor_tensor(out=msk3, in0=lg3, in1=m2.to_broadcast([128, 4, NE]), op=ALU.is_ge),
                  reads=['lg', 'mx8'], writes=['msk'])
            cx.op('dve', lambda e: e.tensor_tensor(out=ex3, in0=ex3, in1=sm[:, :, 3:4].to_broadcast([128, 4, NE]), op=ALU.mult),
                  reads=['ex', 'sm3'], writes=['ex'])
            cx.op('dve', lambda e: e.tensor_tensor(out=gates[:, ti * 4:(ti + 1) * 4, :], in0=ex3, in1=msk3, op=ALU.mult),
                  reads=['ex', 'msk'], writes=[('gates', ti * 4 + b) for b in range(4)])

        def build_gate(e, ti):
            Gb = Gt[e % 2]
            pg = g.psx[1]
            for b in range(4):
                blk = ti * 4 + b
                db = dgt[blk % 2]
                cx.op('dve', lambda en: en.tensor_scalar(out=db[1][:], in0=g.ident[:], scalar1=gates[:, blk, e:e + 1], scalar2=None, op0=ALU.mult),
                      reads=[('gates', blk), 'ident'], writes=[db[0]])
                cx.op('pe', lambda en: en.matmul(pg[1][:, b * 128:(b + 1) * 128], g.ones_f[:], db[1][:], start=True, stop=True),
                      reads=[db[0]], writes=[pg[0]], inc=True)
            cx.op('act', lambda en: en.activation(out=Gb[1][:, ti * 512:(ti + 1) * 512], in_=pg[1][:], func=AF.Copy),
                  reads=[pg[0]], writes=[Gb[0] + (ti,)])

        def stage1(ti):
            emit_rmsnorm(cx, g, V_MNORM, [TILES_REAL[ti]], xn, 'xn', sq, [rstd[0]], [g.psx[1]], xn32=lambda _t: x32, nsq=4)
            router(ti)
            build_gate(0, ti)

        def pre_step(gi, ti):
            if gi == 0 and ti + 1 < 4:
                stage1(ti + 1)

        def on_group(gi):
            e, j = divmod(gi, 7)
            if j == 2 and e + 1 < NE:
                for ti in range(4):
                    build_gate(e + 1, ti)

        stage1(0)
        emit_ffn(cx, g, groups, TILES_REAL, xn, on_group=on_group, pre_step=pre_step, first_slot=slot0)
        cx.barrier()


_CACHE = {}


def _host_inputs(inputs, b):
    x = np.asarray(inputs['x'], dtype=np.float32)
    xT = np.zeros((D, NPOS), np.float32)
    xT[:, PAD:PAD + NMETA] = np.asarray(inputs['meta'], np.float32).T
    xT[:, PAD + NMETA:] = x[b].T
    return xT


def _vecs(inputs):
    def fm(v):
        return np.asarray(v, np.float32).reshape(8, 128).T
    vecs = np.zeros((128, NV), np.float32)
    vecs[:, V_ANORM:V_ANORM + 8] = fm(inputs['a_norm'][0])
    vecs[:, V_FNORM:V_FNORM + 8] = fm(inputs['f_norm'][0])
    vecs[:, V_CNORM:V_CNORM + 8] = fm(inputs['c_norm'][0])
    vecs[:, V_MNORM:V_MNORM + 8] = fm(inputs['m_norm'][0])
    vecs[:, V_CBDW:V_CBDW + 8] = fm(inputs['c_b_dw'][0])
    vecs[:, V_CLNG:V_CLNG + 8] = fm(inputs['c_ln_g'][0])
    vecs[:, V_CLNB:V_CLNB + 8] = fm(inputs['c_ln_b'][0])
    wdw = np.asarray(inputs['c_w_dw'][0], np.float32)
    vecs[:, V_WDW:V_WDW + 248] = wdw.reshape(31, 8, 128).transpose(2, 1, 0).reshape(128, 8 * 31)
    vecs[:, V_QN] = np.tile(np.asarray(inputs['a_q_norm'][0], np.float32), 2)
    vecs[:, V_KN] = np.tile(np.asarray(inputs['a_k_norm'][0], np.float32), 2)
    vecs[:, V_ON] = np.asarray(inputs['a_o_norm'][0], np.float32)
    vecs[:, V_SINK:V_SINK + 8] = np.asarray(inputs['a_sinks'][0], np.float32)[None, :]
    return vecs


def _consts():
    c = np.zeros((128, NC), np.float32)
    i = np.arange(128)
    R = np.zeros((128, 128), np.float32)
    for m in range(128):
        d = m % 64
        if d < 32:
            R[m + 32, m] = -1.0
        else:
            R[m - 32, m] = 1.0
    c[:, C_ROT:C_ROT + 128] = R
    same_half = (i[:, None] // 64) == (i[None, :] // 64)
    tri = (i[:, None] <= i[None, :]) & same_half
    c[:, C_MC:C_MC + 128] = tri.astype(np.float32) * (-1.0 / 16.0)
    c[:, C_GMASK:C_GMASK + 128] = tri.astype(np.float32)
    tri2 = (i[:, None] > i[None, :]) & same_half
    c[:, C_MC2:C_MC2 + 128] = tri2.astype(np.float32) * (-1.0 / 16.0)
    j = i[:, None]
    q = i[None, :]
    m_same = (j <= q)
    m_prev = (j > q)
    valid0 = (j >= PAD)
    for t, m in enumerate([m_same, m_prev, m_same & valid0, m_prev & valid0]):
        c[:, C_SMASK + 512 * t:C_SMASK + 512 * (t + 1)] = np.tile(np.broadcast_to(m, (128, 128)).astype(np.float32), (1, 4))
    inv_freq = 1.0 / (10000.0 ** (np.arange(0, 64, 2, dtype=np.float32) / 64.0))
    pos = (np.arange(NPOS, dtype=np.float32) - PAD)
    ang = pos[None, :] * inv_freq[(i % 64) % 32][:, None]
    c[:, C_COS:C_COS + NPOS] = np.cos(ang)
    c[:, C_SIN:C_SIN + NPOS] = np.sin(ang)
    return c


def make_in_maps(inputs, xTs, names=None):
    shared = {
        "vecs": _vecs(inputs),
        "ident": np.eye(128, dtype=np.float32),
        "consts": _consts(),
        "a_w_in": np.ascontiguousarray(inputs['a_w_in'][0]), "a_w_gate2": np.ascontiguousarray(inputs['a_w_gate2'][0]),
        "a_bgate": np.ascontiguousarray(np.asarray(inputs['a_b_gate'][0], np.float32).reshape(1, 256)),
        "a_w_out": np.ascontiguousarray(inputs['a_w_out'][0]),
        "f_w1": np.ascontiguousarray(inputs['f_w1'][0]), "f_w3": np.ascontiguousarray(inputs['f_w3'][0]),
        "f_w2": np.ascontiguousarray(inputs['f_w2'][0]),
        "c_w_pw1": np.ascontiguousarray(inputs['c_w_pw1'][0]), "c_w_pw2": np.ascontiguousarray(inputs['c_w_pw2'][0]),
        "m_w_router": np.ascontiguousarray(inputs['m_w_router'][0]),
        "m_w1": np.ascontiguousarray(inputs['m_w1'][0]), "m_w3": np.ascontiguousarray(inputs['m_w3'][0]),
        "m_w2": np.ascontiguousarray(inputs['m_w2'][0]),
    }
    if names is not None:
        shared = {k: v for k, v in shared.items() if k in names}
    return [dict(shared, xT=xTs[b]) for b in range(8)]


def kernel(**inputs):
    if 'full' not in _CACHE:
        _CACHE['full'] = build_program("ABCD", debug_out=False)[0]
    nc = _CACHE['full']
    xTs = [_host_inputs(inputs, b) for b in range(8)]
    in_maps = make_in_maps(inputs, xTs)
    res = run_bass_kernel_spmd(nc, in_maps, core_ids=list(range(8)))
    out = np.stack([np.ascontiguousarray(r["outT"].T) for r in res.results], axis=0)
    return out.astype(np.float32)
```
